# Optimizing a Trainium2 kernel written in Bass

```python
import math, functools
import jax, jax.numpy as jnp
from jax import lax
import numpy as np

D_MODEL = 1024
BATCH = 2
SEQ = 8192
DEPTH = 2

HEAD_DIM = 64
N_MIX_HEADS = D_MODEL // HEAD_DIM
HEADS_PER_MIXER = N_MIX_HEADS // 4
GROUP_WIDTH = HEADS_PER_MIXER * HEAD_DIM
Q_BLOCK = 128
MOBA_BLOCK = 256
MOBA_TOPK = 3
MLA_Q_RANK = D_MODEL // 4
MLA_KV_RANK = D_MODEL // 8
MLA_NOPE = HEAD_DIM
MLA_ROPE = HEAD_DIM // 2
MLA_V = HEAD_DIM
ROPE_THETA = 10000.0
DILATED_BRANCHES = ((128, 1), (512, 4), (2048, 16))
SWA_WINDOW = 128
SWA_KV_HEADS = 2
SWA_KV_WIDTH = SWA_KV_HEADS * HEAD_DIM
D_FF = 4 * D_MODEL
EPS = 1e-6
NEG = -1e30
COL_WIDTHS = (GROUP_WIDTH, GROUP_WIDTH, GROUP_WIDTH,
              MLA_Q_RANK, MLA_KV_RANK, MLA_ROPE,
              GROUP_WIDTH, GROUP_WIDTH, GROUP_WIDTH,
              GROUP_WIDTH, SWA_KV_WIDTH, SWA_KV_WIDTH)
IN_COLS = sum(COL_WIDTHS)

kernel_name = "hybrid_parallel_moba_mla_dilated_swa"


def rms_norm(x, g):
    xf = x.astype(jnp.float32)
    y = xf * lax.rsqrt(jnp.mean(xf * xf, axis=-1, keepdims=True) + EPS)
    return (y * g.astype(jnp.float32)).astype(x.dtype)


def split_columns(h):
    parts, off = [], 0
    for wdt in COL_WIDTHS:
        parts.append(h[..., off:off + wdt])
        off += wdt
    return parts


def alibi_slopes():
    n = 3 * HEADS_PER_MIXER
    idx = np.arange(1, n + 1, dtype=np.float32).reshape(HEADS_PER_MIXER, 3)
    s = jnp.asarray(np.exp2(-8.0 * idx / n), dtype=jnp.float32)
    return s[:, 0], s[:, 1], s[:, 2]


def rope_tables(S):
    inv = 1.0 / (ROPE_THETA ** (jnp.arange(0, MLA_ROPE, 2, dtype=jnp.float32) / MLA_ROPE))
    ang = jnp.arange(S, dtype=jnp.float32)[:, None] * inv[None, :]
    return jnp.cos(ang), jnp.sin(ang)


def apply_rope(x, cos, sin):
    x1, x2 = jnp.split(x.astype(jnp.float32), 2, axis=-1)
    c, s = cos[None, :, None, :], sin[None, :, None, :]
    return jnp.concatenate([x1 * c - x2 * s, x1 * s + x2 * c], axis=-1).astype(x.dtype)


def moba_attention(q, k, v, slopes):
    B, H, S, dh = q.shape
    nb = -(-S // MOBA_BLOCK)
    Sp = nb * MOBA_BLOCK
    topk = min(MOBA_TOPK, nb)
    pad = ((0, 0), (0, 0), (0, Sp - S), (0, 0))
    qp, kp, vp = jnp.pad(q, pad), jnp.pad(k, pad), jnp.pad(v, pad)
    kb = kp.reshape(B, H, nb, MOBA_BLOCK, dh)
    vb = vp.reshape(B, H, nb, MOBA_BLOCK, dh)
    kmean = jnp.mean(kb.astype(jnp.float32), axis=3)
    scale = dh ** -0.5
    bi = jnp.arange(B)[:, None, None, None]
    hi = jnp.arange(H)[None, :, None, None]
    slope_b = slopes.astype(jnp.float32)

    def chunk(c):
        t0 = c * Q_BLOCK
        qc = lax.dynamic_slice_in_dim(qp, t0, Q_BLOCK, axis=2)
        tpos = t0 + jnp.arange(Q_BLOCK)
        qblk = tpos // MOBA_BLOCK
        own = t0 // MOBA_BLOCK
        gate = jnp.einsum('bhqd,bhnd->bhqn', qc.astype(jnp.float32), kmean)
        past = jnp.arange(nb)[None, :] < qblk[:, None]
        gate = jnp.where(past, gate, NEG)
        _, sel = lax.top_k(gate, topk)
        sel_ok = sel < qblk[:, None]
        ksel = kb[bi, hi, sel]
        vsel = vb[bi, hi, sel]
        kpos_sel = sel[..., None] * MOBA_BLOCK + jnp.arange(MOBA_BLOCK)
        dist_sel = (tpos[:, None, None] - kpos_sel).astype(jnp.float32)
        s_sel = (jnp.einsum('bhqd,bhqjkd->bhqjk', qc, ksel).astype(jnp.float32) * scale
                 - slope_b[None, :, None, None, None] * dist_sel)
        s_sel = jnp.where(sel_ok[..., None], s_sel, NEG).reshape(B, H, Q_BLOCK, topk * MOBA_BLOCK)
        kown = lax.dynamic_slice_in_dim(kb, own, 1, axis=2)[:, :, 0]
        vown = lax.dynamic_slice_in_dim(vb, own, 1, axis=2)[:, :, 0]
        dist_own = tpos[:, None] - (own * MOBA_BLOCK + jnp.arange(MOBA_BLOCK))[None, :]
        s_own = (jnp.einsum('bhqd,bhkd->bhqk', qc, kown).astype(jnp.float32) * scale
                 - slope_b[None, :, None, None] * dist_own.astype(jnp.float32))
        s_own = jnp.where(dist_own >= 0, s_own, NEG)
        p = jax.nn.softmax(jnp.concatenate([s_sel, s_own], axis=-1), axis=-1)
        p_sel = p[..., :topk * MOBA_BLOCK].reshape(B, H, Q_BLOCK, topk, MOBA_BLOCK)
        p_own = p[..., topk * MOBA_BLOCK:]
        return (jnp.einsum('bhqjk,bhqjkd->bhqd', p_sel.astype(v.dtype), vsel)
                + jnp.einsum('bhqk,bhkd->bhqd', p_own.astype(v.dtype), vown))

    out = lax.map(chunk, jnp.arange(Sp // Q_BLOCK))
    return out.transpose(1, 2, 0, 3, 4).reshape(B, H, Sp, dh)[:, :, :S]


def causal_dense_attention(q, k, v, scale):
    B, H, S, _ = q.shape
    kpos = jnp.arange(S)

    def blk(c):
        qc = lax.dynamic_slice_in_dim(q, c * Q_BLOCK, Q_BLOCK, axis=2)
        s = jnp.einsum('bhqd,bhkd->bhqk', qc, k).astype(jnp.float32) * scale
        tpos = c * Q_BLOCK + jnp.arange(Q_BLOCK)
        s = jnp.where(kpos[None, :] <= tpos[:, None], s, NEG)
        p = jax.nn.softmax(s, axis=-1)
        return jnp.einsum('bhqk,bhkd->bhqd', p.astype(v.dtype), v)

    out = lax.map(blk, jnp.arange(S // Q_BLOCK))
    return out.transpose(1, 2, 0, 3, 4).reshape(B, H, S, -1)


def mla_attention(q_lat, kv_lat, k_rope, g_qlat, g_kvlat, w_uq, w_ukv, g_q, g_k):
    B, S, _ = q_lat.shape
    H = HEADS_PER_MIXER
    qk_dim = MLA_NOPE + MLA_ROPE
    q = (rms_norm(q_lat, g_qlat) @ w_uq).reshape(B, S, H, qk_dim)
    kv = (rms_norm(kv_lat, g_kvlat) @ w_ukv).reshape(B, S, H, MLA_NOPE + MLA_V)
    k_nope, v = kv[..., :MLA_NOPE], kv[..., MLA_NOPE:]
    k = jnp.concatenate([k_nope, jnp.broadcast_to(k_rope[:, :, None, :], (B, S, H, MLA_ROPE))], axis=-1)
    q, k = rms_norm(q, g_q), rms_norm(k, g_k)
    cos, sin = rope_tables(S)
    q = jnp.concatenate([q[..., :MLA_NOPE], apply_rope(q[..., MLA_NOPE:], cos, sin)], axis=-1)
    k = jnp.concatenate([k[..., :MLA_NOPE], apply_rope(k[..., MLA_NOPE:], cos, sin)], axis=-1)
    t = lambda a: a.transpose(0, 2, 1, 3)
    return causal_dense_attention(t(q), t(k), t(v), qk_dim ** -0.5)


def dilated_branch(q, k, v, slopes, window, dilation):
    B, H, S, dh = q.shape
    span = dilation * Q_BLOCK
    Sp = -(-S // span) * span
    L = Sp // dilation
    nb = L // Q_BLOCK
    steps = window // dilation

    def to_sub(t):
        t = jnp.pad(t, ((0, 0), (0, 0), (0, Sp - S), (0, 0)))
        return t.reshape(B, H, L, dilation, dh).transpose(0, 1, 3, 2, 4).reshape(B, H, dilation, nb, Q_BLOCK, dh)

    def with_prev(t):
        prev = jnp.pad(t, ((0, 0), (0, 0), (0, 0), (1, 0), (0, 0), (0, 0)))[:, :, :, :-1]
        return jnp.concatenate([prev, t], axis=4)

    qs = to_sub(q)
    kk, vv = with_prev(to_sub(k)), with_prev(to_sub(v))
    qi = jnp.arange(Q_BLOCK)[:, None]
    kidx = jnp.arange(2 * Q_BLOCK)[None, :]
    rel = qi + Q_BLOCK - kidx
    blk = jnp.arange(nb)[:, None, None]
    valid = (rel >= 0) & (rel <= steps) & (blk * Q_BLOCK + kidx[None] - Q_BLOCK >= 0)
    s = (jnp.einsum('bhrnqd,bhrnkd->bhrnqk', qs, kk).astype(jnp.float32) * (dh ** -0.5)
         - slopes.astype(jnp.float32)[None, :, None, None, None, None] * (dilation * rel).astype(jnp.float32))
    s = jnp.where(valid, s, NEG)
    m = jnp.max(s, axis=-1, keepdims=True)
    e = jnp.exp(s - m)
    l = jnp.sum(e, axis=-1, keepdims=True)
    o = jnp.einsum('bhrnqk,bhrnkd->bhrnqd', e, vv.astype(jnp.float32))
    back = lambda t: t.reshape(B, H, dilation, L, -1).transpose(0, 1, 3, 2, 4).reshape(B, H, Sp, -1)[:, :, :S]
    return back(o), back(m), back(l)


def dilated_mixture(q, k, v, slopes):
    branches = [dilated_branch(q, k, v, slopes, w, d) for (w, d) in DILATED_BRANCHES]
    M = functools.reduce(jnp.maximum, [b[1] for b in branches])
    num = jnp.zeros(q.shape, jnp.float32)
    den = jnp.zeros(q.shape[:-1] + (1,), jnp.float32)
    for o, m, l in branches:
        w = jnp.exp(m - M)
        num = num + w * o
        den = den + w * l
    return (num / den).astype(q.dtype)


def swa_sink_attention(q, k, v, sinks, slopes):
    B, H, S, dh = q.shape
    G = H // SWA_KV_HEADS
    nb = S // Q_BLOCK
    qb = q.reshape(B, SWA_KV_HEADS, G, nb, Q_BLOCK, dh)

    def with_prev(t):
        t = t.reshape(B, SWA_KV_HEADS, nb, Q_BLOCK, dh)
        prev = jnp.pad(t, ((0, 0), (0, 0), (1, 0), (0, 0), (0, 0)))[:, :, :-1]
        return jnp.concatenate([prev, t], axis=3)

    kk, vv = with_prev(k), with_prev(v)
    qi = jnp.arange(Q_BLOCK)[:, None]
    kidx = jnp.arange(2 * Q_BLOCK)[None, :]
    rel = qi + Q_BLOCK - kidx
    blk = jnp.arange(nb)[:, None, None]
    valid = (rel >= 0) & (rel < SWA_WINDOW) & (blk * Q_BLOCK + kidx[None] - Q_BLOCK >= 0)
    sl = slopes.astype(jnp.float32).reshape(SWA_KV_HEADS, G)[None, :, :, None, None, None]
    s = jnp.einsum('bkgnqd,bkncd->bkgnqc', qb, kk).astype(jnp.float32) * (dh ** -0.5) - sl * rel.astype(jnp.float32)
    s = jnp.where(valid, s, NEG)
    sink = jnp.broadcast_to(sinks.astype(jnp.float32).reshape(SWA_KV_HEADS, G)[None, :, :, None, None, None],
                            s.shape[:-1] + (1,))
    p = jax.nn.softmax(jnp.concatenate([s, sink], axis=-1), axis=-1)[..., :-1]
    o = jnp.einsum('bkgnqc,bkncd->bkgnqd', p.astype(v.dtype), vv)
    return o.reshape(B, H, S, dh)


def hybrid_layer(x, attn_norm_g, w_in, moba_q_g, moba_k_g, mla_qlat_g, mla_kvlat_g,
                 mla_w_uq, mla_w_ukv, mla_q_g, mla_k_g, dil_q_g, dil_k_g, swa_q_g, swa_k_g,
                 swa_sinks, group_out_g, w_o, mlp_norm_g, w_up, w_down):
    B, S, _ = x.shape
    H = HEADS_PER_MIXER
    slope_a, slope_c, slope_d = alibi_slopes()
    h = rms_norm(x, attn_norm_g) @ w_in
    a_q, a_k, a_v, b_ql, b_kvl, b_kr, c_q, c_k, c_v, d_q, d_k, d_v = split_columns(h)
    heads = lambda t, n: t.reshape(B, S, n, HEAD_DIM)
    tr = lambda t: t.transpose(0, 2, 1, 3)
    oa = moba_attention(tr(rms_norm(heads(a_q, H), moba_q_g)), tr(rms_norm(heads(a_k, H), moba_k_g)),
                        tr(heads(a_v, H)), slope_a)
    ob = mla_attention(b_ql, b_kvl, b_kr, mla_qlat_g, mla_kvlat_g, mla_w_uq, mla_w_ukv, mla_q_g, mla_k_g)
    oc = dilated_mixture(tr(rms_norm(heads(c_q, H), dil_q_g)), tr(rms_norm(heads(c_k, H), dil_k_g)),
                         tr(heads(c_v, H)), slope_c)
    od = swa_sink_attention(tr(rms_norm(heads(d_q, H), swa_q_g)),
                            tr(rms_norm(heads(d_k, SWA_KV_HEADS), swa_k_g)),
                            tr(heads(d_v, SWA_KV_HEADS)), swa_sinks, slope_d)
    groups = [tr(o).reshape(B, S, GROUP_WIDTH) for o in (oa, ob, oc, od)]
    mixed = jnp.concatenate([rms_norm(gr, group_out_g[i]) for i, gr in enumerate(groups)], axis=-1)
    x = x + mixed @ w_o
    u = jax.nn.relu(rms_norm(x, mlp_norm_g) @ w_up)
    return x + (u * u) @ w_down


def setup_inputs(seed: int = 0) -> dict:
    key = jax.random.key(seed)
    ks = jax.random.split(key, 24)
    f = jnp.float32
    H = HEADS_PER_MIXER
    w = lambda k, shape, fan_in: jax.random.normal(k, shape, f) * (fan_in ** -0.5)
    g = lambda k, shape: 1.0 + 0.02 * jax.random.normal(k, shape, f)
    return {
        "x": jax.random.normal(ks[0], (BATCH, SEQ, D_MODEL), f),
        "attn_norm_g": g(ks[1], (DEPTH, D_MODEL)),
        "w_in": w(ks[2], (DEPTH, D_MODEL, IN_COLS), D_MODEL),
        "moba_q_g": g(ks[3], (DEPTH, HEAD_DIM)),
        "moba_k_g": g(ks[4], (DEPTH, HEAD_DIM)),
        "mla_qlat_g": g(ks[5], (DEPTH, MLA_Q_RANK)),
        "mla_kvlat_g": g(ks[6], (DEPTH, MLA_KV_RANK)),
        "mla_w_uq": w(ks[7], (DEPTH, MLA_Q_RANK, H * (MLA_NOPE + MLA_ROPE)), MLA_Q_RANK),
        "mla_w_ukv": w(ks[8], (DEPTH, MLA_KV_RANK, H * (MLA_NOPE + MLA_V)), MLA_KV_RANK),
        "mla_q_g": g(ks[9], (DEPTH, MLA_NOPE + MLA_ROPE)),
        "mla_k_g": g(ks[10], (DEPTH, MLA_NOPE + MLA_ROPE)),
        "dil_q_g": g(ks[11], (DEPTH, HEAD_DIM)),
        "dil_k_g": g(ks[12], (DEPTH, HEAD_DIM)),
        "swa_q_g": g(ks[13], (DEPTH, HEAD_DIM)),
        "swa_k_g": g(ks[14], (DEPTH, HEAD_DIM)),
        "swa_sinks": 0.5 * jax.random.normal(ks[15], (DEPTH, H), f),
        "group_out_g": g(ks[16], (DEPTH, 4, GROUP_WIDTH)),
        "w_o": w(ks[17], (DEPTH, D_MODEL, D_MODEL), D_MODEL),
        "mlp_norm_g": g(ks[18], (DEPTH, D_MODEL)),
        "w_up": w(ks[19], (DEPTH, D_MODEL, D_FF), D_MODEL),
        "w_down": w(ks[20], (DEPTH, D_FF, D_MODEL), D_FF),
    }


def reference(x, attn_norm_g, w_in, moba_q_g, moba_k_g, mla_qlat_g, mla_kvlat_g, mla_w_uq,
              mla_w_ukv, mla_q_g, mla_k_g, dil_q_g, dil_k_g, swa_q_g, swa_k_g, swa_sinks,
              group_out_g, w_o, mlp_norm_g, w_up, w_down):
    for l in range(DEPTH):
        x = hybrid_layer(x, attn_norm_g[l], w_in[l], moba_q_g[l], moba_k_g[l], mla_qlat_g[l],
                         mla_kvlat_g[l], mla_w_uq[l], mla_w_ukv[l], mla_q_g[l], mla_k_g[l],
                         dil_q_g[l], dil_k_g[l], swa_q_g[l], swa_k_g[l], swa_sinks[l],
                         group_out_g[l], w_o[l], mlp_norm_g[l], w_up[l], w_down[l])
    return x
```

```python
import contextlib
import numpy as np
import ml_dtypes
import concourse.bass as bass
import concourse.mybir as mybir
from concourse.bass_utils import run_bass_kernel_spmd

F32 = mybir.dt.float32
BF16 = mybir.dt.bfloat16
AF = mybir.ActivationFunctionType
ALU = mybir.AluOpType
AX = mybir.AxisListType
NPBF = ml_dtypes.bfloat16

S = 8192
D = 1024
EPS = 1e-6
BIG = 240000.0
NCORES = 8


class Buf:
    __slots__ = ("name", "w", "r", "multi")

    def __init__(self, name, multi=False):
        self.name = name
        self.w = {}
        self.r = {}
        self.multi = multi


class Op:
    __slots__ = ("eng", "fn", "deps", "marked", "val", "dma", "sem", "semval", "uid")


class Prog:
    ENG = ("pe", "act", "dve", "pool", "sp")
    NDSEM = 8

    def __init__(self, nc, stack):
        self.nc = nc
        self.ops = {e: [] for e in self.ENG}
        self.esem = {e: stack.enter_context(nc.semaphore("es_" + e)) for e in self.ENG}
        self.dsem = {e: [stack.enter_context(nc.semaphore("ds_%s%d" % (e, i))) for i in range(self.NDSEM)]
                     for e in ("sp", "pool", "act")}
        self.dsem["cc"] = [stack.enter_context(nc.semaphore("ds_cc%d" % i)) for i in range(4)]
        self.dtot = {e: [0] * len(v) for e, v in self.dsem.items()}
        self.drr = {e: 0 for e in self.dsem}
        self.uid = 0

    def add(self, eng, fn, r=(), w=(), dma=False, cc=False):
        op = Op()
        op.eng, op.fn, op.dma, op.marked, op.val = eng, fn, dma, False, 0
        self.uid += 1
        op.uid = self.uid
        deps = set()
        for b in r:
            deps.update(b.w.values())
        for b in w:
            deps.update(b.r.values())
            if not b.multi:
                deps.update(b.w.values())
        key = ("d", op.uid) if dma else eng
        for b in r:
            b.r[key] = op
        for b in w:
            if b.multi:
                b.w[key] = op
            else:
                b.w = {key: op}
                b.r = {}
        if eng == "pe":
            deps = {d for d in deps if d.dma or d.eng != "pe"}
        op.deps = deps
        for d in deps:
            if not d.dma:
                d.marked = True
        if dma:
            se = "cc" if cc else eng
            k = self.drr[se]
            self.drr[se] = (k + 1) % len(self.dsem[se])
            op.sem = (se, k)
            self.dtot[se][k] += 1 if cc else 16
            op.semval = self.dtot[se][k]
            op.val = 1 if cc else 16
        self.ops[eng].append(op)
        return op

    def emit(self):
        nc = self.nc
        if not hasattr(self, "cnt"):
            self.cnt = {e: 0 for e in self.ENG}
            self.waited = {e: {} for e in self.ENG}
            self.prev_last = {e: 0 for e in self.ENG}
            self.prev_dtot = {e: [0] * len(v) for e, v in self.dtot.items()}
        for e in self.ENG:
            last = None
            for op in self.ops[e]:
                if not op.dma:
                    last = op
            if last is not None:
                last.marked = True
            for op in self.ops[e]:
                if op.marked and not op.dma:
                    self.cnt[e] += 1
                    op.val = self.cnt[e]
        names = {"pe": "tensor", "act": "scalar", "dve": "vector", "pool": "gpsimd", "sp": "sync"}
        prev_last = dict(self.prev_last)
        prev_dtot = {e: list(v) for e, v in self.prev_dtot.items()}
        with nc.Block() as block:
            for e in self.ENG:
                def body(eng, e=e):
                    waited = self.waited[e]

                    def wait(sem, key, val):
                        if waited.get(key, 0) < val:
                            eng.wait_ge(sem, val)
                            waited[key] = val
                    for e2 in self.ENG:
                        if e2 != e and prev_last[e2]:
                            wait(self.esem[e2], e2, prev_last[e2])
                    for e2, tots in prev_dtot.items():
                        if e2 == "cc":
                            continue
                        for k, tot in enumerate(tots):
                            if tot:
                                wait(self.dsem[e2][k], (e2, k), tot)
                    self.cur_eng = e
                    for op in self.ops[e]:
                        for d in op.deps:
                            if d.dma:
                                wait(self.dsem[d.sem[0]][d.sem[1]], d.sem, d.semval)
                            else:
                                wait(self.esem[d.eng], d.eng, d.val)
                        if op.dma:
                            wait(self.dsem[op.sem[0]][op.sem[1]], op.sem, op.semval - op.val)
                        ins = op.fn(eng)
                        if op.dma:
                            if op.val == 1:
                                ins.then_inc(self.dsem[op.sem[0]][op.sem[1]])
                            else:
                                ins.then_inc(self.dsem[op.sem[0]][op.sem[1]], 16)
                        elif op.marked:
                            ins.then_inc(self.esem[e], 1)
                    for e2 in self.dsem:
                        own = (e2 == e)
                        if own:
                            for k in range(len(self.dsem[e2])):
                                if self.dtot[e2][k]:
                                    wait(self.dsem[e2][k], (e2, k), self.dtot[e2][k])
                getattr(block, names[e])(body)
        for e in self.ENG:
            self.prev_last[e] = self.cnt[e]
            self.ops[e] = []
        self.prev_dtot = {e: list(v) for e, v in self.dtot.items()}


class Ring:
    def __init__(self, items):
        self.items = items
        self.i = 0

    def next(self):
        it = self.items[self.i]
        self.i = (self.i + 1) % len(self.items)
        return it


class Ctx:
    def __init__(self, nc, stack):
        self.nc, self.stack = nc, stack
        self.P = Prog(nc, stack)
        self.n = 0
        self._pid = {}

    def pid(self, e, fn, tag):
        ph = self.P.cnt["pe"] if hasattr(self.P, "cnt") else -1
        k0 = (id(e), ph)
        if k0 not in self._pid:
            self._pid[k0] = e.partition_id()
        k = (id(e), ph, tag)
        if k not in self._pid:
            self._pid[k] = fn(self._pid[k0])
        return self._pid[k]

    def sb(self, shape, dt, name=None, multi=False):
        self.n += 1
        nm = "%s_%d" % (name or "t", self.n)
        t = self.stack.enter_context(self.nc.sbuf_tensor(nm, list(shape), dt))
        return t, Buf(nm, multi)

    def sbring(self, k, shape, dt, name=None):
        return Ring([self.sb(shape, dt, name) for _ in range(k)])

    def ps(self, dt=F32, name=None):
        self.n += 1
        nm = "%s_%d" % (name or "ps", self.n)
        shape = [128, 512] if dt == F32 else [128, 1024]
        t = self.stack.enter_context(self.nc.psum_tensor(nm, shape, dt))
        return t, Buf(nm)

    def dram(self, name, shape, dt, kind):
        return self.nc.dram_tensor(name, list(shape), dt, kind=kind).ap()

    def dma(self, eng, out, in_, r=(), w=()):
        return self.P.add(eng, lambda e: e.dma_start(out=out, in_=in_), r, w, dma=True)

    def mm(self, out, lhsT, rhs, start, stop, r, w):
        return self.P.add("pe", lambda e: e.matmul(out, lhsT, rhs, start=start, stop=stop), r, w)

    def tr(self, out, in_, ident, r, w):
        return self.P.add("pe", lambda e: e.transpose(out, in_, ident), r, w)

    def act(self, out, in_, func, r, w, bias=0.0, scale=1.0, eng="act"):
        return self.P.add(eng, lambda e: e.activation(out, in_, func, bias=bias, scale=scale), r, w)

    def copy(self, eng, out, in_, r, w):
        if eng == "act":
            return self.P.add("act", lambda e: e.copy(out, in_), r, w)
        return self.P.add(eng, lambda e: e.tensor_copy(out, in_), r, w)

    def tt(self, eng, out, in0, in1, op, r, w):
        return self.P.add(eng, lambda e: e.tensor_tensor(out, in0, in1, op), r, w)

    def ts(self, eng, out, in0, s1, op0, r, w, s2=None, op1=None):
        if op1 is None:
            return self.P.add(eng, lambda e: e.tensor_scalar(out, in0, s1, None, op0), r, w)
        return self.P.add(eng, lambda e: e.tensor_scalar(out, in0, s1, s2, op0, op1), r, w)

    def stt(self, eng, out, in0, scalar, in1, op0, op1, r, w):
        return self.P.add(eng, lambda e: e.scalar_tensor_tensor(out, in0, scalar, in1, op0, op1), r, w)

    def recip(self, out, in_, r, w):
        return self.P.add("dve", lambda e: e.reciprocal(out, in_), r, w)

    def memset(self, eng, ap, val, w):
        return self.P.add(eng, lambda e: e.memset(ap, val), (), w)


def phase_M(C, G, l, do_layer, do_norm, x_d, x_db, xo_d, xo_db):
    out_x = xo_d is not None
    with contextlib.ExitStack() as st:
        C.stack = st
        id_d = G["ident"]
        if do_layer:
            ot_all = G["OT_all"]
            wo_d, gog_d, mlg_d, wup_d, wdn_d = (G["%s_%d" % (n, l)] for n in ("w_o", "gog", "mlg", "w_up", "w_down"))
        xnT_d = G["xnT_own"]

        X, _ = C.sb([128, 16, D], F32, "X")
        Xb = [Buf("X%d" % i) for i in range(16)]
        ident, identb = C.sb([128, 128], BF16, "ident")
        C.dma("sp", ident[:, :], id_d[:, :], (), (identb,))
        xv = x_d.rearrange("(b p) d -> p b d", p=128)
        for tb in range(16):
            C.dma("sp" if tb % 2 == 0 else "pool", X[:, tb, :], xv[:, tb, :], (x_db,), (Xb[tb],))
        AT, _ = C.sb([128, 8, 2048], BF16, "AT")
        ATb = [Buf("AT%d" % i) for i in range(16)]
        pst = [C.ps(BF16, "pst") for _ in range(2)]
        pstr = Ring(pst)
        psf = Ring([C.ps(F32, "psf") for _ in range(6)])
        sq_r = C.sbring(2, [128, D], F32, "sq")
        xnb_r = C.sbring(2, [128, D], BF16, "xnb")
        st_r = C.sbring(4, [128, 4], F32, "stat")

        def norm_T(tb, dstT, dstb):
            sq, sqb = sq_r.next()
            C.act(sq[:, :], X[:, tb, :], AF.Square, (Xb[tb],), (sqb,))
            s, sb_ = st_r.next()
            C.P.add("dve", lambda e: e.reduce_sum(s[:, 0:1], sq[:, :], AX.X), (sqb,), (sb_,))
            C.act(s[:, 1:2], s[:, 0:1], AF.Sqrt, (sb_,), (sb_,), bias=EPS, scale=1.0 / D)
            C.recip(s[:, 2:3], s[:, 1:2], (sb_,), (sb_,))
            xnb, xnbb = xnb_r.next()
            C.ts("dve", xnb[:, :], X[:, tb, :], s[:, 2:3], ALU.mult, (Xb[tb], sb_), (xnbb,))
            pt, ptb = pstr.next()
            for kc in range(8):
                C.tr(pt[:, kc * 128:(kc + 1) * 128], xnb[:, kc * 128:(kc + 1) * 128], ident[:, :],
                     (xnbb, identb), (ptb,))
            C.copy("act", dstT[:, :, tb * 128:(tb + 1) * 128],
                   pt[:, :].rearrange("p (k t) -> p k t", k=8), (ptb,), (dstb,))

        if do_layer:
            gog, gogb = C.sb([128, 8], F32, "gog")
            mlg, mlgb = C.sb([128, 8], F32, "mlg")
            C.dma("sp", gog[:, :], gog_d[:, :], (), (gogb,))
            C.dma("sp", mlg[:, :], mlg_d[:, :], (), (mlgb,))
            ones, onesb = C.sb([128, 1], BF16, "ones")
            C.memset("pool", ones[:, :], 1.0, (onesb,))
            if True:
                WU, wob = C.sb([128, 8192], BF16, "WU", multi=True)
                wo = WU[:, :].rearrange("p (k n) -> p k n", k=8)
                stg_r = C.sbring(2, [128, D], F32, "wostg")
                wov = wo_d.rearrange("(k p) n -> p k n", p=128)
                for kc in range(8):
                    sg, sgb = stg_r.next()
                    C.dma("sp", sg[:, :], wov[:, kc, :], (), (sgb,))
                    C.P.add("act", lambda e, kc=kc, sg=sg: e.activation(wo[:, kc, :], sg[:, :], AF.Copy, scale=gog[:, kc:kc + 1]),
                            (sgb, gogb), (wob,))
                for m in range(4):
                    otv = ot_all[m].rearrange("(a r) t -> r a t", a=2)
                    for b2 in range(2):
                        def f(e, m=m, b2=b2, otv=otv):
                            off = C.pid(e, lambda p: (p % 4) * 2048, "i4")
                            return e.dma_start(out=AT[b2 * 64:(b2 + 1) * 64, 2 * m:2 * m + 2, :],
                                               in_=otv[b2 * 64:(b2 + 1) * 64, :, bass.ds(off, 2048)])
                        C.P.add("sp", f, (G["OT_all_b"][m],), tuple(ATb), dma=True)
                sqo_r = C.sbring(2, [128, 8, 128], BF16, "sqo")
                for tb in range(16):
                    tsl = slice(tb * 128, (tb + 1) * 128)
                    sqo, sqob = sqo_r.next()
                    C.tt("pool", sqo[:, :, :], AT[:, :, tsl], AT[:, :, tsl], ALU.mult, (ATb[tb],), (sqob,))
                    pss, pssb = psf.next()
                    for g in range(4):
                        for j in range(2):
                            C.mm(pss[:, g:g + 1], sqo[:, 2 * g + j, :], ones[:, :], j == 0, j == 1,
                                 (sqob, onesb), (pssb,))
                    s, sb_ = st_r.next()
                    rs, rsb = st_r.next()
                    C.act(s[:, 0:4], pss[:, 0:4], AF.Sqrt, (pssb,), (sb_,), bias=EPS, scale=1.0 / 256)
                    C.recip(rs[:, 0:4], s[:, 0:4], (sb_,), (rsb,))
                    for half in range(2):
                        hs = slice(half * 512, (half + 1) * 512)
                        for g in range(4):
                            pg, pgb = psf.next()
                            for j in range(2):
                                C.mm(pg[:, :], AT[:, 2 * g + j, tsl], wo[:, 2 * g + j, hs], j == 0, j == 1,
                                     (ATb[tb], wob), (pgb,))
                            C.stt("dve", X[:, tb, hs], pg[:, :], rs[:, g:g + 1], X[:, tb, hs], ALU.mult, ALU.add,
                                  (pgb, rsb, Xb[tb]), (Xb[tb],))
            if True:
                for tb in range(16):
                    norm_T(tb, AT, ATb[tb])
                ustg_r = C.sbring(3, [128, 2, 512], F32, "ustg")
                dstg_r = C.sbring(2, [128, 1024], F32, "dstg")
                wup_r = Ring([C.sb([128, 8, 512], BF16, "wup", multi=True) for _ in range(2)])
                wdn_r = Ring([C.sb([128, 4, D], BF16, "wdn", multi=True) for _ in range(2)])
                uT = WU[:, :].rearrange("p (k n) -> p k n", k=4)
                uTb = [[Buf("uT%d_%d" % (a, b)) for b in range(4)] for a in range(4)]
                sq2_r = C.sbring(2, [128, 512], F32, "sq2")
                wupv = wup_d.rearrange("(k p) n -> p k n", p=128)
                wdnv = wdn_d.rearrange("(k p) n -> p k n", p=128)
                for fg in range(8):
                    wup, wupb = wup_r.next()
                    wdn, wdnb = wdn_r.next()
                    for k2 in range(4):
                        sg, sgb = ustg_r.next()
                        C.dma("sp", sg[:, :, :], wupv[:, 2 * k2:2 * k2 + 2, fg * 512:(fg + 1) * 512], (), (sgb,))
                        for j in range(2):
                            kc = 2 * k2 + j
                            C.P.add("act", lambda e, kc=kc, sg=sg, j=j, wup=wup: e.activation(
                                wup[:, kc, :], sg[:, j, :], AF.Copy, scale=mlg[:, kc:kc + 1]), (sgb, mlgb), (wupb,))
                    for fc in range(4):
                        sg, sgb = dstg_r.next()
                        C.dma("pool", sg[:, :], wdnv[:, fg * 4 + fc, :], (), (sgb,))
                        C.copy("act" if fc % 2 else "dve", wdn[:, fc, :], sg[:, :], (sgb,), (wdnb,))
                    for fc in range(4):
                        for tg in range(4):
                            pu, pub = psf.next()
                            for kc in range(8):
                                C.mm(pu[:, :], wup[:, kc, fc * 128:(fc + 1) * 128],
                                     AT[:, kc, tg * 512:(tg + 1) * 512], kc == 0, kc == 7,
                                     (wupb,) + tuple(ATb[4 * tg:4 * tg + 4]), (pub,))
                            sq2, sq2b = sq2_r.next()
                            C.act(sq2[:, :], pu[:, :], AF.Square, (pub,), (sq2b,))
                            C.stt("dve", uT[:, fc, tg * 512:(tg + 1) * 512], pu[:, :], 0.0, sq2[:, :],
                                  ALU.is_gt, ALU.mult, (pub, sq2b), (uTb[fc][tg], wob))
                    for tb in range(16):
                        tsl = slice(tb * 128, (tb + 1) * 128)
                        for half in range(2):
                            hs = slice(half * 512, (half + 1) * 512)
                            pd, pdb = psf.next()
                            for fc in range(4):
                                C.mm(pd[:, :], uT[:, fc, tsl], wdn[:, fc, hs], fc == 0, fc == 3,
                                     (uTb[fc][tb // 4], wdnb), (pdb,))
                            C.tt("dve", X[:, tb, hs], pd[:, :], X[:, tb, hs], ALU.add, (pdb, Xb[tb]), (Xb[tb],))
        if out_x:
            xov = xo_d.rearrange("(b p) d -> p b d", p=128)
            for tb in range(16):
                C.dma("sp" if tb % 2 == 0 else "pool", xov[:, tb, :], X[:, tb, :], (Xb[tb],), (xo_db,))
        if do_norm:
            for q in range(4):
                for tb in range(4 * q, 4 * q + 4):
                    norm_T(tb, AT, ATb[tb])
                xnv = xnT_d[q].rearrange("(k p) t -> p k t", p=128)
                C.dma("sp", xnv, AT[:, :, q * 512:(q + 1) * 512],
                      tuple(ATb[4 * q:4 * q + 4]), (G["xnT_own_b"][q],))
                allgather(C, G["xnT_own_t"][q], G["xnT_own_b"][q], G["xnT_all_t"][q], G["xnT_all_b"][q])
        C.P.emit()


def allgather(C, src_t, src_b, dst_t, dst_b):
    C.P.add("pool", lambda e: e.collective_compute(
        "AllGather", ALU.bypass, replica_groups=[[0, 1, 2, 3], [4, 5, 6, 7]],
        ins=[src_t.ap().opt()], outs=[dst_t.ap().opt()]), (src_b,), (dst_b,), dma=True, cc=True)


NAB = 640
NCD = 384


A_LAYER_INPUTS = (("ang", [128, 8], F32), ("wAB", [D, NAB], F32), ("wCD", [D, NCD], F32), ("wuq", [256, 192], F32),
                  ("gql", [128, 2], F32), ("wukv", [128, 128], F32), ("gkvl", [128, 1], F32), ("gcol", [128, 8], F32),
                  ("sink", [128, 1], F32))
A_SHARED_INPUTS = (("rope", [64, S], F32), ("T2", [128, 128], F32), ("PM", [128, 128], F32), ("kbias", [128, 2], F32),
                   ("OH", [32, S], BF16), ("cst", [128, 512], BF16), ("Wt", [128, 1024], BF16), ("SEL", [128, 64], F32))


def phase_A(C, G, l):
    with contextlib.ExitStack() as st:
        C.stack = st
        (ang_d, wAB_d, wCD_d, wuq_d, gql_d, wukv_d, gkvl_d, gcol_d, esk_d) = (G["%s_%d" % (n, l)] for n, _, _ in A_LAYER_INPUTS)
        (rope_d, T2_d, PM_d, kb_d, OH_d, cst_d, W_d, sel_d) = (G[n] for n, _, _ in A_SHARED_INPUTS)
        xnT_all, xnT_all_b = G["xnT_all"], G["xnT_all_b"]
        OT_d, OT_b = G["OT_own"], G["OT_own_b"]

        ld = lambda shape, dt, src, nm, eng="sp": _load(C, shape, dt, src, nm, eng)
        ang, angb = ld([128, 8], F32, ang_d[:, :], "ang")
        gql, gqlb = ld([128, 2], F32, gql_d[:, :], "gql")
        gkvl, gkvlb = ld([128, 1], F32, gkvl_d[:, :], "gkvl")
        gcol, gcolb = ld([128, 8], F32, gcol_d[:, :], "gcol")
        T2, T2b = ld([128, 128], F32, T2_d[:, :], "T2")
        PM, PMb = ld([128, 128], F32, PM_d[:, :], "PM")
        kbias, kbb = ld([128, 2], F32, kb_d[:, :], "kbias")
        cst, cstb = ld([128, 512], BF16, cst_d[:, :], "cst")
        Wt, Wtb = ld([128, 1024], BF16, W_d[:, :], "Wt")
        SEL, SELb = ld([128, 64], F32, sel_d[:, :], "SEL")
        esk, eskb = ld([128, 1], F32, esk_d[:, :], "esk")
        C.act(esk[:, :], esk[:, :], AF.Exp, (eskb,), (eskb,))
        epsc, epsb = C.sb([128, 1], F32, "epsc")
        C.memset("pool", epsc[:, :], EPS, (epsb,))
        ident = cst[:, 0:128]
        TRI = cst[:, 128:256]
        BD = cst[:, 256:384]
        ONES = cst[:, 384:512]

        T1, T1b = C.sb([128, S], BF16, "T1", multi=True)
        T2t, T2tb = C.sb([128, S], BF16, "T2t", multi=True)
        T3, T3b = C.sb([128, S], BF16, "T3", multi=True)
        T4, T4b = C.sb([128, S], BF16, "T4", multi=True)
        Vt = [C.sb([128, 64, 65], BF16, "V%d" % i, multi=True) for i in range(4)]
        for (v, vb) in Vt:
            C.memset("pool", v[:, :, :], 1.0, (vb,))
        RAWOCT, _ = C.sb([128, 6 * 512], F32, "RAWOCT")
        OCT, OCTb = RAWOCT[:, 0:2048], Buf("OCT")
        raw_items = [(RAWOCT[:, i * 512:(i + 1) * 512], Buf("raw%d" % i)) for i in range(6)]

        wAB, wABb = C.sb([128, 8, NAB], BF16, "wAB", multi=True)
        wCD, wCDb = C.sb([128, 8, NCD], BF16, "wCD", multi=True)
        wuq, wuqb = C.sb([128, 2, 192], BF16, "wuq", multi=True)
        wukv, wukvb = C.sb([128, 128], BF16, "wukv")
        wABv = wAB_d.rearrange("(k p) n -> p k n", p=128)
        wCDv = wCD_d.rearrange("(k p) n -> p k n", p=128)
        wuqv = wuq_d.rearrange("(k p) n -> p k n", p=128)
        wload = []

        def stage_w(dst, dstb, src, ncols, scol, scolb, nk, q):
            for kc in range(nk):
                for c0 in range(0, ncols, 512):
                    n = min(512, ncols - c0)
                    sg, sgb = stg_ring.next()
                    sv = src[:, kc, c0:c0 + n] if nk > 1 or len(src.shape) == 3 else src[:, c0:c0 + n]
                    dv = dst[:, kc, c0:c0 + n] if len(dst.shape) == 3 else dst[:, c0:c0 + n]
                    C.dma(q, sg[:, 0:n], sv, (), (sgb,))
                    C.ts("dve", dv, sg[:, 0:n], scol[:, kc:kc + 1], ALU.mult, (sgb, scolb), (dstb,))

        psA = Ring([C.ps(F32, "psA") for _ in range(4)])
        psB = Ring([C.ps(F32, "psB") for _ in range(2)])
        psC = Ring([C.ps(F32, "psC") for _ in range(1)])
        psT, psTb = C.ps(BF16, "psT")
        xn_r = C.sbring(2, [128, 8, 512], BF16, "xn")
        f_r = C.sbring(6, [128, 512], F32, "f")
        qr_r = C.sbring(4, [128, 512], F32, "qr")
        vt_r = C.sbring(4, [128, 512], BF16, "vt")
        h_r = C.sbring(4, [128, 512], BF16, "h")
        rp_r = C.sbring(2, [128, 2, 512], F32, "rp")
        xnv4 = [a.rearrange("(r k p) t -> r p k t", r=4, p=128) for a in xnT_all]

        def load_xn(c):
            xn, xnb = xn_r.next()
            src = xnv4[c % 4][c // 4]
            C.dma("sp", xn[:, 0:4, :], src[:, 0:4, :], (xnT_all_b[c % 4],), (xnb,))
            C.dma("pool", xn[:, 4:8, :], src[:, 4:8, :], (xnT_all_b[c % 4],), (xnb,))
            return xn, xnb
        stg_ring = Ring(f_r.items + raw_items)
        stage_w(wAB, wABb, wABv, NAB, ang, angb, 8, "act")
        stage_w(wuq, wuqb, wuqv, 192, gql, gqlb, 2, "act")
        stage_w(wukv, wukvb, wukv_d, 128, gkvl, gkvlb, 1, "act")
        ksum, ksumb = C.sb([64, 32], F32, "ksum")
        C_qlb = C.sbring(4, [128, 2, 512], BF16, "ql")

        def proj(xn, xnb, c0, ncol):
            p, pb = psA.next()
            for kc in range(8):
                C.mm(p[0:ncol, :], wsrc[0][:, kc, c0:c0 + ncol], xn[:, kc, :], kc == 0, kc == 7,
                     (wsrc[1], xnb), (pb,))
            return p, pb

        def rstd_of(src, srcb, lo, hi, ones_ap, n, extra=None):
            h, hb = h_r.next()
            C.act(h[lo:hi, :], src[lo:hi, :], AF.Square, (srcb,), (hb,))
            p, pb = psB.next()
            C.mm(p[lo:hi, :], ones_ap, h[lo:hi, :], True, True, (hb, cstb), (pb,))
            f, fb = f_r.next()
            C.act(f[lo:hi, :], p[lo:hi, :], AF.Ln, (pb, epsb), (fb,), bias=epsc[lo:hi, 0:1], scale=1.0 / n)
            C.act(f[lo:hi, :], f[lo:hi, :], AF.Exp, (fb,), (fb,), scale=-0.5)
            return f, fb

        def vtrans(vT, vTb, base, tok0, stride, V, Vb, blk0, nblk):
            for j in range(nblk):
                a = tok0 + j * 128 * stride
                C.tr(psT[:, j * 64:(j + 1) * 64], vT[base:base + 64, a:a + 127 * stride + 1:stride],
                     cst[base:base + 64, base:base + 64], (vTb, cstb), (psTb,))
            C.copy("act", V[:, blk0:blk0 + nblk, 0:64],
                   psT[:, 0:nblk * 64].rearrange("p (j d) -> p j d", j=nblk), (psTb,), (Vb,))

        QA, KA, QB, KB = T1, T2t, T3, T4
        VA, VAb = Vt[0]
        VB, VBb = Vt[1]
        C.dma("sp", KA[64:96, :], OH_d[:, :], (), (T2tb,))
        wsrc = [wAB, wABb]

        raw_r = Ring(raw_items)
        hk_r = C.sbring(4, [128, 512], BF16, "hk")

        def stat(h_ap, hb, ones_ap, lo, hi, n, k2=None):
            p, pb = psB.next()
            if k2 is None:
                C.mm(p[lo:hi, :], ones_ap, h_ap, True, True, (hb, cstb), (pb,))
            else:
                for j in range(2):
                    C.mm(p[lo:hi, :], ones_ap, k2[:, j, :], j == 0, j == 1, (hb, cstb), (pb,))
            f, fb = f_r.next()
            C.act(f[lo:hi, :], p[lo:hi, :], AF.Ln, (pb, epsb), (fb,), bias=epsc[lo:hi, 0:1], scale=1.0 / n)
            C.act(f[lo:hi, :], f[lo:hi, :], AF.Exp, (fb,), (fb,), scale=-0.5)
            return f, fb

        def stageA(c, mid=None):
            cs = slice(c * 512, (c + 1) * 512)
            xn, xnb = load_xn(c)
            rp, rpb = rp_r.next()
            C.dma("sp", rp[64:96, 0, :], rope_d[0:32, cs], (), (rpb,))
            C.dma("sp", rp[64:96, 1, :], rope_d[32:64, cs], (), (rpb,))
            raws = []
            for (c0, n) in ((0, 96), (96, 64), (160, 96)):
                p, pb = proj(xn, xnb, c0, n)
                rw, rwb = raw_r.next()
                C.copy("act", rw[0:n, :], p[0:n, :], (pb,), (rwb,))
                raws.append((rw, rwb))
            if mid is not None:
                mid()
            qlb, qlbb = C_qlb.next()
            hq, hqb = C_qlb.next()
            for j in range(2):
                p, pb = proj(xn, xnb, 256 + 128 * j, 128)
                C.copy("act", qlb[:, j, :], p[:, :], (pb,), (qlbb,))
                C.tt("dve", hq[:, j, :], p[:, :], qlb[:, j, :], ALU.mult, (pb, qlbb), (hqb,))
            p5, p5b = proj(xn, xnb, 512, 128)
            kvb, kvbb = hk_r.next()
            hkv, hkvb = hk_r.next()
            C.copy("act", kvb[:, :], p5[:, :], (p5b,), (kvbb,))
            C.tt("dve", hkv[:, :], p5[:, :], kvb[:, :], ALU.mult, (p5b, kvbb), (hkvb,))
            (r3, r3b) = raws[2]
            rl, rlb = stat(None, hqb, ONES, 0, 128, 256, k2=hq)
            rkv, rkvb = stat(hkv[:, :], hkvb, ONES, 0, 128, 128)
            P1, P1b = psC.next()
            for j in range(2):
                C.mm(P1[0:96, :], wuq[:, j, 0:96], qlb[:, j, :], j == 0, j == 1, (wuqb, qlbb), (P1b,))
            QR, QRb = qr_r.next()
            C.tt("dve", QR[0:96, :], P1[0:96, :], rl[0:96, :], ALU.mult, (P1b, rlb), (QRb,))
            P2, P2b = psC.next()
            for j in range(2):
                C.mm(P2[0:96, :], wuq[:, j, 96:192], qlb[:, j, :], j == 0, j == 1, (wuqb, qlbb), (P2b,))
            QRP, QRPb = qr_r.next()
            C.tt("dve", QRP[64:96, :], P2[64:96, :], rl[64:96, :], ALU.mult, (P2b, rlb), (QRPb,))
            vT, vTb = vt_r.next()
            C.copy("pool", vT[0:64, :], r3[0:64, :], (r3b,), (vTb,))
            Pk, Pkb = psC.next()
            C.mm(Pk[0:64, :], wukv[:, 0:64], kvb[:, :], True, True, (wukvb, kvbb), (Pkb,))
            C.tt("dve", r3[0:64, :], Pk[0:64, :], rkv[0:64, :], ALU.mult, (Pkb, rkvb, vTb), (r3b,))
            Pv, Pvb = psC.next()
            C.mm(Pv[0:64, :], wukv[:, 64:128], kvb[:, :], True, True, (wukvb, kvbb), (Pvb,))
            vT2, vT2b = vt_r.next()
            C.tt("dve", vT2[0:64, :], Pv[0:64, :], rkv[0:64, :], ALU.mult, (Pvb, rkvb), (vT2b,))
            return dict(c=c, rp=(rp, rpb), raws=raws, QR=(QR, QRb), QRP=(QRP, QRPb), vT=(vT, vTb), vT2=(vT2, vT2b))

        def stageB(S_):
            c = S_["c"]
            cs = slice(c * 512, (c + 1) * 512)
            rp, rpb = S_["rp"]
            (r1, r1b), (r2, r2b), (r3, r3b) = S_["raws"]
            QR, QRb = S_["QR"]
            QRP, QRPb = S_["QRP"]
            vT, vTb = S_["vT"]
            vT2, vT2b = S_["vT2"]
            vtrans(vT, vTb, 0, 0, 1, VA, VAb, 4 * c, 4)
            vtrans(vT2, vT2b, 0, 0, 1, VB, VBb, 4 * c, 4)
            hs = []
            for (src, srcb, lo, hi) in ((r1, r1b, 0, 64), (r2, r2b, 0, 64), (QR, QRb, 0, 96), (r3, r3b, 0, 96)):
                h, hb = h_r.next()
                C.act(h[lo:hi, :], src[lo:hi, :], AF.Square, (srcb,), (hb,))
                hs.append((h, hb))
            rsq, rsqb = stat(hs[0][0][0:64, :], hs[0][1], ONES[0:64, 0:64], 0, 64, 64)
            rsk, rskb = stat(hs[1][0][0:64, :], hs[1][1], ONES[0:64, 0:64], 0, 64, 64)
            rq, rqb = stat(hs[2][0][0:96, :], hs[2][1], ONES[0:96, 0:96], 0, 96, 96)
            rk, rkb = stat(hs[3][0][0:96, :], hs[3][1], ONES[0:96, 0:96], 0, 96, 96)
            C.stt("dve", QA[0:64, cs], r1[0:64, :], gcol[0:64, 0:1], rsq[0:64, :], ALU.mult, ALU.mult,
                  (r1b, gcolb, rsqb), (T1b,))
            C.stt("dve", r2[0:64, :], r2[0:64, :], gcol[0:64, 1:2], rsk[0:64, :], ALU.mult, ALU.mult,
                  (r2b, gcolb, rskb), (r2b,))
            C.copy("pool", KA[0:64, cs], r2[0:64, :], (r2b,), (T2tb,))
            C.P.add("dve", lambda e, r2=r2, c=c: e.reduce_sum(
                ksum[0:64, 2 * c:2 * c + 2], r2[0:64, :].rearrange("p (a b) -> p a b", a=2), AX.X),
                (r2b,), (ksumb,))
            C.stt("dve", QB[0:64, cs], QR[0:64, :], gcol[0:64, 2:3], rq[0:64, :], ALU.mult, ALU.mult,
                  (QRb, gcolb, rqb), (T3b,))
            C.stt("dve", QR[64:96, :], QR[64:96, :], gcol[64:96, 2:3], rp[64:96, 0, :], ALU.mult, ALU.mult,
                  (QRb, gcolb, rpb), (QRb,))
            C.stt("dve", QRP[64:96, :], QRP[64:96, :], gcol[64:96, 3:4], rp[64:96, 1, :], ALU.mult, ALU.mult,
                  (QRPb, gcolb, rpb), (QRPb,))
            C.tt("pool", QR[64:96, :], QR[64:96, :], QRP[64:96, :], ALU.add, (QRb, QRPb), (QRb,))
            C.tt("pool", QB[64:96, cs], QR[64:96, :], rq[64:96, :], ALU.mult, (QRb, rqb), (T3b,))
            C.stt("dve", KB[0:64, cs], r3[0:64, :], gcol[0:64, 4:5], rk[0:64, :], ALU.mult, ALU.mult,
                  (r3b, gcolb, rkb), (T4b,))
            C.stt("dve", r3[64:96, :], r3[64:96, :], gcol[64:96, 4:5], rp[64:96, 0, :], ALU.mult, ALU.mult,
                  (r3b, gcolb, rpb), (r3b,))
            C.stt("dve", r1[64:96, :], r1[64:96, :], gcol[64:96, 5:6], rp[64:96, 1, :], ALU.mult, ALU.mult,
                  (r1b, gcolb, rpb), (r1b,))
            C.tt("pool", r3[64:96, :], r3[64:96, :], r1[64:96, :], ALU.add, (r3b, r1b), (r3b,))
            C.tt("pool", KB[64:96, cs], r3[64:96, :], rk[64:96, :], ALU.mult, (r3b, rkb), (T4b,))

        prevS = None
        for c in [r * 4 + j for j in range(4) for r in range(4)]:
            curS = stageA(c, (lambda p=prevS: stageB(p)) if prevS is not None else None)
            prevS = curS
        stageB(prevS)

        stg_ring = Ring(f_r.items)
        stage_w(wCD, wCDb, wCDv, NCD, ang, angb, 8, "sp")

        E_r = C.sbring(4, [128, 512], BF16, "E")
        Osb_r = C.sbring(2, [128, 512], F32, "Osb")
        oo_r = C.sbring(2, [64, 512], BF16, "oo")

        def finalize(Osrc, Osrcb, m, col0, sink=False):
            pd, pdb = psC.next()
            C.mm(pd[0:64, :], SEL[0:65, 0:64], Osrc, True, True, (Osrcb, SELb), (pdb,))
            rc, rcb = f_r.next()
            if sink:
                C.ts("dve", rc[0:64, :], pd[0:64, :], esk[0:64, 0:1], ALU.add, (pdb, eskb), (rcb,))
                C.recip(rc[0:64, :], rc[0:64, :], (rcb,), (rcb,))
            else:
                C.recip(rc[0:64, :], pd[0:64, :], (pdb,), (rcb,))
            oo, oob = oo_r.next()
            C.tt("dve", oo[0:64, :], Osrc[0:64, :], rc[0:64, :], ALU.mult, (Osrcb, rcb), (oob,))
            C.dma("sp", OT_d[m][:, col0:col0 + 512], oo[0:64, :], (oob,), (OT_b[m],))

        class Task:
            __slots__ = ("qk", "ex", "pv", "newO", "fin", "after")

        def run_tasks(tasks, look=3):
            n = len(tasks)
            Sx = [None] * n

            def emit_qk(i):
                Sx[i] = psA.next()
                tasks[i].qk(*Sx[i])
            for i in range(min(look, n)):
                emit_qk(i)
            pend = []
            O = None
            for i, t in enumerate(tasks):
                if i + look < n:
                    emit_qk(i + look)
                E = E_r.next()
                t.ex(Sx[i][0], Sx[i][1], E[0], E[1])
                if t.newO:
                    O = psB.next()
                t.pv(E[0], E[1], O[0], O[1])
                for f in pend:
                    f()
                pend = []
                if t.fin is not None:
                    pend.append(lambda t=t, O=O: t.fin(O[0], O[1]))
                for f in (t.after or ()):
                    f()
            for f in pend:
                f()

        def dense_tasks(Kt, Ktb, Qt, Qtb, rows, V, Vb, scale, bias_of, m):
            tasks = []
            for qt in range(16):
                nkt = 4 * qt + 4
                for kt in range(nkt):
                    j = kt - 4 * qt
                    c0 = 128 * j if j > 0 else 0
                    t = Task()

                    def qk(Sx, Sb, kt=kt, qt=qt, c0=c0):
                        C.mm(Sx[:, c0:512], Kt[0:rows, kt * 128:(kt + 1) * 128],
                             Qt[0:rows, qt * 512 + c0:(qt + 1) * 512], True, True, (Ktb, Qtb), (Sb,))

                    def ex(Sx, Sb, E, Eb, kt=kt, c0=c0, j=j):
                        b = bias_of(kt)
                        rd = (Sb,) if isinstance(b, float) else (Sb, kbb)
                        C.act(E[:, c0:512], Sx[:, c0:512], AF.Exp, rd, (Eb,), bias=b, scale=scale)
                        if j >= 0:
                            C.tt("dve", E[:, c0:c0 + 128], E[:, c0:c0 + 128], TRI, ALU.mult, (Eb, cstb), (Eb,))

                    def pv(E, Eb, O, Ob, kt=kt, c0=c0, nkt=nkt):
                        C.mm(O[0:65, c0:512], V[:, kt, :], E[:, c0:512], kt == 0, kt == nkt - 1, (Vb, Eb), (Ob,))

                    def fin(O, Ob, qt=qt):
                        Osb, Osbb = Osb_r.next()
                        C.copy("act", Osb[0:65, :], O[0:65, :], (Ob,), (Osbb,))
                        finalize(Osb[0:65, :], Osbb, m, qt * 512)
                    t.qk, t.ex, t.pv = qk, ex, pv
                    t.newO = kt == 0
                    t.fin = fin if kt == nkt - 1 else None
                    t.after = None
                    tasks.append(t)
            return tasks

        km, kmb = C.sb([64, 32], BF16, "km")
        C.ts("dve", km[:, :], ksum[:, :], 1.0 / 256, ALU.mult, (ksumb,), (kmb,))
        STg_r = C.sbring(3, [128, 128], BF16, "STg")
        for (stg_, stgb_) in STg_r.items:
            C.memset("pool", stg_[:, :], 0.0, (stgb_,))
        g_r = C.sbring(4, [128, 80], F32, "g")

        def gphase1(qb):
            qblk = qb // 2
            qs = slice(qb * 128, (qb + 1) * 128)
            pg, pgb = psC.next()
            C.mm(pg[:, 0:32], QA[0:64, qs], km[0:64, :], True, True, (T1b, kmb), (pgb,))
            g, gb = g_r.next()
            C.tt("dve", g[:, 0:32], pg[:, 0:32], PM[:, 32 - qblk:64 - qblk], ALU.add, (pgb, PMb), (gb,))
            C.P.add("dve", lambda e: e.max(g[:, 64:72], g[:, 0:32]), (gb,), (gb,))
            C.ts("dve", g[:, 72:73], g[:, 66:67], -1e29, ALU.max, (gb,), (gb,))
            C.ts("dve", g[:, 32:64], g[:, 0:32], g[:, 72:73], ALU.is_ge, (gb,), (gb,))
            C.tt("dve", g[:, 32:64], g[:, 32:64], PM[:, 64 + 32 - qblk:64 + 64 - qblk], ALU.add, (gb, PMb), (gb,))
            C.tt("dve", g[:, 0:32], g[:, 32:64], T2[:, 63 - qb:63 - qb + 64:2], ALU.mult, (gb, T2b), (gb,))
            stg, stgb = STg_r.next()
            C.ts("dve", stg[:, 64:96], g[:, 0:32], -BIG, ALU.add, (gb,), (stgb,))

            def part2():
                C.tr(psT[:, 0:128], stg[:, :], ident, (stgb, cstb), (psTb,))
                C.copy("act", QA[64:96, qs], psT[64:96, 0:128], (psTb,), (T1b,))
            return part2

        def ag_m(m):
            allgather(C, G["OT_own_t"][m], OT_b[m], G["OT_all_t"][m], G["OT_all_b"][m])

        sB = 96 ** -0.5
        tB = dense_tasks(KB, T4b, QB, T3b, 96, VB, VBb, sB, lambda kt: 0.0, 1)
        step = len(tB) // 64
        for qb in range(64):
            i1 = qb * step
            i2 = min(i1 + 5, len(tB) - 1)
            holder = {}

            def f1(qb=qb, holder=holder):
                holder["p2"] = gphase1(qb)

            def f2(holder=holder):
                holder["p2"]()
            tB[i1].after = (tB[i1].after or ()) + (f1,)
            tB[i2].after = (tB[i2].after or ()) + (f2,)
        run_tasks(tB)
        ag_m(1)
        run_tasks(dense_tasks(KA, T2tb, QA, T1b, 96, VA, VAb, 0.125, lambda kt: kbias[:, kt % 2:kt % 2 + 1], 0))
        ag_m(0)

        QCD, KCD, VCDT = T1, T2t, T3
        wsrc[0], wsrc[1] = wCD, wCDb
        VC1, VC1b = Vt[0]
        VC4, VC4b = Vt[1]
        VC16, VC16b = Vt[2]
        VD, VDb = Vt[3]
        for c in range(16):
            cs = slice(c * 512, (c + 1) * 512)
            xn, xnb = load_xn(c)
            for gi, (dst, dstb) in enumerate(((QCD, T1b), (KCD, T2tb))):
                p, pb = proj(xn, xnb, 128 * gi, 128)
                rs, rsb = rstd_of(p, pb, 0, 128, BD, 64)
                C.stt("dve", dst[:, cs], p[:, :], gcol[:, 6 + gi:7 + gi], rs[:, :], ALU.mult, ALU.mult,
                      (pb, gcolb, rsb), (dstb,))
            p, pb = proj(xn, xnb, 256, 128)
            C.copy("act", VCDT[:, cs], p[:, :], (pb,), (T3b,))
            vtrans(VCDT, T3b, 0, c * 512, 1, VC1, VC1b, 4 * c, 4)
            vtrans(VCDT, T3b, 64, c * 512, 1, VD, VDb, 4 * c, 4)
            for r in range(4):
                vtrans(VCDT, T3b, 0, c * 512 + r, 4, VC4, VC4b, r * 16 + c, 1)
            if c % 4 == 3:
                s_ = c // 4
                for r in range(16):
                    vtrans(VCDT, T3b, 0, s_ * 2048 + r, 16, VC16, VC16b, r * 4 + s_, 1)

        def local(base, d, r, nb, n0, nblk, V, Vb, vidx, Wc, fin):
            tasks = []
            kts = list(range(max(n0 - 1, 0), n0 + nblk))
            for kt in kts:
                qlo, qhi = max(kt, n0), min(kt + 1, n0 + nblk - 1)
                ncol = (qhi - qlo + 1) * 128
                woff = 0 if qlo == kt else 128
                ka = kt * 128 * d + r
                qa = qlo * 128 * d + r
                t = Task()

                def qk(Sx, Sb, ka=ka, qa=qa, ncol=ncol):
                    C.mm(Sx[:, 0:ncol], KCD[base:base + 64, ka:ka + 127 * d + 1:d],
                         QCD[base:base + 64, qa:qa + (ncol - 1) * d + 1:d], True, True, (T2tb, T1b), (Sb,))

                def ex(Sx, Sb, E, Eb, ncol=ncol, woff=woff):
                    C.act(E[:, 0:ncol], Sx[:, 0:ncol], AF.Exp, (Sb,), (Eb,), scale=0.125)
                    C.tt("dve", E[:, 0:ncol], E[:, 0:ncol], Wt[:, Wc + woff:Wc + woff + ncol], ALU.mult,
                         (Eb, Wtb), (Eb,))

                def pv(E, Eb, O, Ob, kt=kt, qlo=qlo, qhi=qhi):
                    for qb in range(qlo, qhi + 1):
                        first = (kt == kts[0]) and (qb == qlo)
                        last = (kt == kts[-1]) and (qb == qhi)
                        C.mm(O[0:65, (qb - n0) * 128:(qb - n0 + 1) * 128], V[:, vidx(kt), :],
                             E[:, (qb - qlo) * 128:(qb - qlo + 1) * 128], first, last, (Vb, Eb), (Ob,))
                t.qk, t.ex, t.pv = qk, ex, pv
                t.newO = kt == kts[0]
                t.fin = fin if kt == kts[-1] else None
                t.after = None
                tasks.append(t)
            return tasks

        tD = []
        for qt in range(16):
            def finD(O, Ob, qt=qt):
                Osb, Osbb = Osb_r.next()
                C.copy("act", Osb[0:65, :], O[0:65, :], (Ob,), (Osbb,))
                finalize(Osb[0:65, :], Osbb, 3, qt * 512, sink=True)
            tD += local(64, 1, 0, 64, 4 * qt, 4, VD, VDb, lambda kt: kt, 768, finD)
        run_tasks(tD)
        ag_m(3)

        for s_ in range(4):
            tC = []

            def acc(O, Ob, off, d, ncol):
                dst = OCT[0:65, off:off + (ncol - 1) * d + 1:d]
                C.tt("dve", dst, O[0:65, 0:ncol], dst, ALU.add, (Ob, OCTb), (OCTb,))
            for q4 in range(4):
                def fin1(O, Ob, q4=q4):
                    C.copy("act", OCT[0:65, q4 * 512:(q4 + 1) * 512], O[0:65, :], (Ob,),
                           (OCTb,) + tuple(rb for _, rb in raw_items))
                tC += local(0, 1, 0, 64, 16 * s_ + 4 * q4, 4, VC1, VC1b, lambda kt: kt, 0, fin1)
            for r in range(4):
                tC += local(0, 4, r, 16, 4 * s_, 4, VC4, VC4b, lambda kt, r=r: r * 16 + kt, 256,
                            lambda O, Ob, r=r: acc(O, Ob, r, 4, 512))
            for r in range(16):
                tC += local(0, 16, r, 4, s_, 1, VC16, VC16b, lambda kt, r=r: r * 4 + kt, 512,
                            lambda O, Ob, r=r: acc(O, Ob, r, 16, 128))
            run_tasks(tC)
            for q4 in range(4):
                finalize(OCT[0:65, q4 * 512:(q4 + 1) * 512], OCTb, 2, s_ * 2048 + q4 * 512)
        ag_m(2)
        C.P.emit()


def _load(C, shape, dt, src, nm, eng="sp"):
    t, b = C.sb(shape, dt, nm)
    C.dma(eng, t[tuple(slice(None) for _ in shape)], src, (), (b,))
    return t, b


M_LAYER_INPUTS = (("w_o", [D, D], F32), ("gog", [128, 8], F32), ("mlg", [128, 8], F32),
                  ("w_up", [D, 4096], F32), ("w_down", [4096, D], F32))


_NEEDED = set()


def build_fused(nphase=5):
    nc = bass.Bass("TRN2", target_bir_lowering=False)
    with contextlib.ExitStack() as st:
        C = Ctx(nc, st)
        G = {}
        x_d = C.dram("x", [2048, D], F32, "ExternalInput")
        G["ident"] = C.dram("ident", [128, 128], BF16, "ExternalInput")
        for l in range(2):
            if nphase >= 3 + 2 * l:
                for n, shp, dt in M_LAYER_INPUTS:
                    G["%s_%d" % (n, l)] = C.dram("%s_%d" % (n, l), shp, dt, "ExternalInput")
            if nphase >= 2 + 2 * l:
                for n, shp, dt in A_LAYER_INPUTS:
                    G["%s_%d" % (n, l)] = C.dram("%s_%d" % (n, l), shp, dt, "ExternalInput")
        if nphase >= 2:
            for n, shp, dt in A_SHARED_INPUTS:
                G[n] = C.dram(n, shp, dt, "ExternalInput")
        _NEEDED.clear()
        _NEEDED.update(k for k in G)
        _NEEDED.update(("x", "ident"))
        xo_d = C.dram("xo", [2048, D], F32, "ExternalOutput")
        t = nc.dram_tensor("xres", [2048, D], F32)
        G["xres"], G["xres_b"] = t.ap(), Buf("xres", multi=True)
        for n, shp in (("xnT_own", [D, 512]), ("xnT_all", [4 * D, 512]), ("OT_own", [64, S]), ("OT_all", [256, S])):
            ts_ = [nc.dram_tensor("%s%d" % (n, j), list(shp), BF16) for j in range(4)]
            G[n + "_t"] = ts_
            G[n] = [t.ap() for t in ts_]
            G[n + "_b"] = [Buf("%s%d" % (n, j), multi=True) for j in range(4)]
        xin_b, xo_b = Buf("xin"), Buf("xo", multi=True)
        phase_M(C, G, 0, False, True, x_d, xin_b, None, None)
        if nphase >= 2:
            phase_A(C, G, 0)
        if nphase >= 3:
            phase_M(C, G, 0, True, True, x_d, xin_b, G["xres"], G["xres_b"])
        if nphase >= 4:
            phase_A(C, G, 1)
        if nphase >= 5:
            phase_M(C, G, 1, True, False, G["xres"], G["xres_b"], xo_d, xo_b)
        else:
            C.stack = st
            C.dma("sp", xo_d[0:128, :], x_d[0:128, :], (), (xo_b,))
            C.P.emit()
    return nc


_CACHE = {}


def _prog(key, fn):
    if key not in _CACHE:
        _CACHE[key] = fn()
    return _CACHE[key]


def _consts(i):
    f8 = np.float64
    sl_a = 2.0 ** (-8.0 * (3 * i + 1) / 12)
    sl_c = 2.0 ** (-8.0 * (3 * i + 2) / 12)
    sl_d = 2.0 ** (-8.0 * (3 * i + 3) / 12)
    p = np.arange(128, dtype=f8)[:, None]
    j = np.arange(128, dtype=f8)[None, :]
    T2 = (-8.0 * sl_a * (128.0 * (63 - j) + p - 128.0) + BIG).astype(np.float32)
    PM = np.zeros((128, 128), np.float32)
    PM[:, 32:64] = -1e30
    PM[:, 64 + 32] = 1.0
    kbias = np.stack([sl_a * (par * 128 + np.arange(128, dtype=f8) - 128) for par in (0, 1)], 1).astype(np.float32)
    k = np.arange(128)[:, None]
    q = np.arange(128)[None, :]
    cst = np.zeros((128, 512), np.float32)
    cst[:, 0:128] = np.eye(128)
    cst[:, 128:256] = (k <= q)
    cst[:, 256:384] = (k // 64 == q // 64)
    cst[:, 384:512] = 1.0
    Wt = np.zeros((128, 1024), f8)
    for bi, d in enumerate((1, 4, 16)):
        Wt[:, bi * 256:bi * 256 + 128] = (k <= q) * np.exp(-sl_c * d * np.maximum(q - k, 0))
        Wt[:, bi * 256 + 128:bi * 256 + 256] = (k >= q) * np.exp(-sl_c * d * (128 + q - k))
    Wt[:, 768:896] = (k <= q) * np.exp(-sl_d * np.maximum(q - k, 0))
    Wt[:, 896:1024] = (k > q) * np.exp(-sl_d * (128 + q - k))
    SEL = np.zeros((128, 64), np.float32)
    SEL[64, :] = 1.0
    return dict(T2=T2, PM=PM, kbias=kbias, cst=cst.astype(NPBF), Wt=Wt.astype(np.float32).astype(NPBF), SEL=SEL)


def _shared_consts():
    inv = 1.0 / (10000.0 ** (np.arange(0, 32, 2, dtype=np.float32) / 32))
    ang = np.arange(S, dtype=np.float32)[:, None] * inv[None, :]
    cos, sin = np.cos(ang).T, np.sin(ang).T
    rope = np.concatenate([cos, cos, -sin, sin], 0).astype(np.float32)
    OH = (np.arange(S)[None, :] // 256 == np.arange(32)[:, None]).astype(np.float32).astype(NPBF)
    return np.ascontiguousarray(rope), np.ascontiguousarray(OH)


def _col(v, n=128):
    o = np.zeros((128,), np.float32)
    o[:len(v)] = v
    return o


def _attn_inputs(P, l, i, xnT_b, rope, OH):
    w_in = P["w_in"][l]
    kv = i // 2
    sel = lambda a, n=64: list(range(a, a + n))
    krp = list(range(1152 + 16, 1184)) + list(range(1152, 1168))
    colsAB = (sel(64 * i) + krp + sel(256 + 64 * i) + sel(512 + 64 * i) + sel(1152, 32)
              + sel(768, 256) + sel(1024, 128))
    colsCD = (sel(1184 + 64 * i) + sel(1952 + 64 * i) + sel(1440 + 64 * i) + sel(2208 + 64 * kv)
              + sel(1696 + 64 * i) + sel(2336 + 64 * kv))
    uq = P["mla_w_uq"][l][:, 96 * i:96 * i + 96]
    perm = list(range(80, 96)) + list(range(64, 80))
    wuq = np.concatenate([uq, uq[:, 0:64], uq[:, perm]], 1)
    gq, gk = P["mla_q_g"][l], P["mla_k_g"][l]
    gqp, gkp = np.zeros(96, np.float32), np.zeros(96, np.float32)
    gqp[64:96], gkp[64:96] = gq[perm], gk[perm]
    gcol = np.stack([_col(P["moba_q_g"][l]), _col(P["moba_k_g"][l]), _col(gq), _col(gqp), _col(gk), _col(gkp),
                     np.concatenate([P["dil_q_g"][l], P["swa_q_g"][l]]),
                     np.concatenate([P["dil_k_g"][l], P["swa_k_g"][l]])], 1).astype(np.float32)
    d = dict(ang=np.ascontiguousarray(P["attn_norm_g"][l].reshape(8, 128).T),
             wAB=np.ascontiguousarray(w_in[:, colsAB]), wCD=np.ascontiguousarray(w_in[:, colsCD]),
             wuq=np.ascontiguousarray(wuq), gql=np.ascontiguousarray(P["mla_qlat_g"][l].reshape(2, 128).T),
             wukv=np.ascontiguousarray(P["mla_w_ukv"][l][:, 128 * i:128 * i + 128]),
             gkvl=np.ascontiguousarray(P["mla_kvlat_g"][l].reshape(128, 1)), gcol=np.ascontiguousarray(gcol),
             rope=rope, OH=OH, sink=np.full((128, 1), P["swa_sinks"][l][i], np.float32))
    d.update(_consts(i))
    return d


def kernel(**inputs):
    P = {k: np.asarray(v) for k, v in inputs.items()}
    x = np.ascontiguousarray(P["x"], dtype=np.float32)
    cores = list(range(NCORES))
    ident = np.eye(128, dtype=np.float32).astype(NPBF)
    rope, OH = _shared_consts()
    nc = _prog("fused", build_fused)
    ims = []
    for c in cores:
        b, i = c // 4, c % 4
        d = {"x": np.ascontiguousarray(x[b, 2048 * i:2048 * (i + 1)]), "ident": ident}
        for l in range(2):
            a = _attn_inputs(P, l, i, None, rope, OH)
            for n, _, _ in A_LAYER_INPUTS:
                d["%s_%d" % (n, l)] = a[n]
            for n, _, _ in A_SHARED_INPUTS:
                d[n] = a[n]
            d["w_o_%d" % l] = P["w_o"][l]
            d["gog_%d" % l] = np.ascontiguousarray(P["group_out_g"][l].reshape(8, 128).T)
            d["mlg_%d" % l] = np.ascontiguousarray(P["mlp_norm_g"][l].reshape(8, 128).T)
            d["w_up_%d" % l] = P["w_up"][l]
            d["w_down_%d" % l] = P["w_down"][l]
        ims.append({k: v for k, v in d.items() if k in _NEEDED})
    res = run_bass_kernel_spmd(nc, ims, core_ids=cores)
    out = np.zeros((2, S, D), np.float32)
    for c in cores:
        out[c // 4, 2048 * (c % 4):2048 * (c % 4 + 1)] = np.asarray(res.results[c]["xo"])
    return out
```

```python
import contextlib
import numpy as np
import ml_dtypes
import concourse.bass as bass
import concourse.mybir as mybir
from concourse.bass_utils import run_bass_kernel_spmd

F32 = mybir.dt.float32
BF16 = mybir.dt.bfloat16
AF = mybir.ActivationFunctionType
ALU = mybir.AluOpType
AX = mybir.AxisListType
NPBF = ml_dtypes.bfloat16

S = 8192
D = 1024
EPS = 1e-6
BIG = 240000.0
NCORES = 8


class Buf:
    __slots__ = ("name", "w", "r", "multi")

    def __init__(self, name, multi=False):
        self.name = name
        self.w = {}
        self.r = {}
        self.multi = multi


class Op:
    __slots__ = ("eng", "fn", "deps", "marked", "val", "dma", "sem", "semval", "uid")


class Prog:
    ENG = ("pe", "act", "dve", "pool", "sp")
    NDSEM = 8

    def __init__(self, nc, stack):
        self.nc = nc
        self.ops = {e: [] for e in self.ENG}
        self.esem = {e: stack.enter_context(nc.semaphore("es_" + e)) for e in self.ENG}
        self.dsem = {e: [stack.enter_context(nc.semaphore("ds_%s%d" % (e, i))) for i in range(self.NDSEM)]
                     for e in ("sp", "pool", "act")}
        self.dsem["cc"] = [stack.enter_context(nc.semaphore("ds_cc%d" % i)) for i in range(4)]
        self.dtot = {e: [0] * len(v) for e, v in self.dsem.items()}
        self.drr = {e: 0 for e in self.dsem}
        self.uid = 0

    def add(self, eng, fn, r=(), w=(), dma=False, cc=False):
        op = Op()
        op.eng, op.fn, op.dma, op.marked, op.val = eng, fn, dma, False, 0
        self.uid += 1
        op.uid = self.uid
        deps = set()
        for b in r:
            deps.update(b.w.values())
        for b in w:
            deps.update(b.r.values())
            if not b.multi:
                deps.update(b.w.values())
        key = ("d", op.uid) if dma else eng
        for b in r:
            b.r[key] = op
        for b in w:
            if b.multi:
                b.w[key] = op
            else:
                b.w = {key: op}
                b.r = {}
        if eng == "pe":
            deps = {d for d in deps if d.dma or d.eng != "pe"}
        op.deps = deps
        for d in deps:
            if not d.dma:
                d.marked = True
        if dma:
            se = "cc" if cc else eng
            k = self.drr[se]
            self.drr[se] = (k + 1) % len(self.dsem[se])
            op.sem = (se, k)
            self.dtot[se][k] += 1 if cc else 16
            op.semval = self.dtot[se][k]
            op.val = 1 if cc else 16
        self.ops[eng].append(op)
        return op

    def emit(self):
        nc = self.nc
        if not hasattr(self, "cnt"):
            self.cnt = {e: 0 for e in self.ENG}
            self.waited = {e: {} for e in self.ENG}
            self.prev_last = {e: 0 for e in self.ENG}
            self.prev_dtot = {e: [0] * len(v) for e, v in self.dtot.items()}
        for e in self.ENG:
            last = None
            for op in self.ops[e]:
                if not op.dma:
                    last = op
            if last is not None:
                last.marked = True
            for op in self.ops[e]:
                if op.marked and not op.dma:
                    self.cnt[e] += 1
                    op.val = self.cnt[e]
        names = {"pe": "tensor", "act": "scalar", "dve": "vector", "pool": "gpsimd", "sp": "sync"}
        prev_last = dict(self.prev_last)
        prev_dtot = {e: list(v) for e, v in self.prev_dtot.items()}
        with nc.Block() as block:
            for e in self.ENG:
                def body(eng, e=e):
                    waited = self.waited[e]

                    def wait(sem, key, val):
                        if waited.get(key, 0) < val:
                            eng.wait_ge(sem, val)
                            waited[key] = val
                    for e2 in self.ENG:
                        if e2 != e and prev_last[e2]:
                            wait(self.esem[e2], e2, prev_last[e2])
                    for e2, tots in prev_dtot.items():
                        if e2 == "cc":
                            continue
                        for k, tot in enumerate(tots):
                            if tot:
                                wait(self.dsem[e2][k], (e2, k), tot)
                    self.cur_eng = e
                    for op in self.ops[e]:
                        for d in op.deps:
                            if d.dma:
                                wait(self.dsem[d.sem[0]][d.sem[1]], d.sem, d.semval)
                            else:
                                wait(self.esem[d.eng], d.eng, d.val)
                        if op.dma:
                            wait(self.dsem[op.sem[0]][op.sem[1]], op.sem, op.semval - op.val)
                        ins = op.fn(eng)
                        if op.dma:
                            if op.val == 1:
                                ins.then_inc(self.dsem[op.sem[0]][op.sem[1]])
                            else:
                                ins.then_inc(self.dsem[op.sem[0]][op.sem[1]], 16)
                        elif op.marked:
                            ins.then_inc(self.esem[e], 1)
                    for e2 in self.dsem:
                        own = (e2 == e)
                        if own:
                            for k in range(len(self.dsem[e2])):
                                if self.dtot[e2][k]:
                                    wait(self.dsem[e2][k], (e2, k), self.dtot[e2][k])
                getattr(block, names[e])(body)
        for e in self.ENG:
            self.prev_last[e] = self.cnt[e]
            self.ops[e] = []
        self.prev_dtot = {e: list(v) for e, v in self.dtot.items()}


class Ring:
    def __init__(self, items):
        self.items = items
        self.i = 0

    def next(self):
        it = self.items[self.i]
        self.i = (self.i + 1) % len(self.items)
        return it


class Ctx:
    def __init__(self, nc, stack):
        self.nc, self.stack = nc, stack
        self.P = Prog(nc, stack)
        self.n = 0
        self._pid = {}

    def pid(self, e, fn, tag):
        ph = self.P.cnt["pe"] if hasattr(self.P, "cnt") else -1
        k0 = (id(e), ph)
        if k0 not in self._pid:
            self._pid[k0] = e.partition_id()
        k = (id(e), ph, tag)
        if k not in self._pid:
            self._pid[k] = fn(self._pid[k0])
        return self._pid[k]

    def sb(self, shape, dt, name=None, multi=False):
        self.n += 1
        nm = "%s_%d" % (name or "t", self.n)
        t = self.stack.enter_context(self.nc.sbuf_tensor(nm, list(shape), dt))
        return t, Buf(nm, multi)

    def sbring(self, k, shape, dt, name=None):
        return Ring([self.sb(shape, dt, name) for _ in range(k)])

    def ps(self, dt=F32, name=None):
        self.n += 1
        nm = "%s_%d" % (name or "ps", self.n)
        shape = [128, 512] if dt == F32 else [128, 1024]
        t = self.stack.enter_context(self.nc.psum_tensor(nm, shape, dt))
        return t, Buf(nm)

    def dram(self, name, shape, dt, kind):
        return self.nc.dram_tensor(name, list(shape), dt, kind=kind).ap()

    def dma(self, eng, out, in_, r=(), w=()):
        return self.P.add(eng, lambda e: e.dma_start(out=out, in_=in_), r, w, dma=True)

    def mm(self, out, lhsT, rhs, start, stop, r, w):
        return self.P.add("pe", lambda e: e.matmul(out, lhsT, rhs, start=start, stop=stop), r, w)

    def tr(self, out, in_, ident, r, w):
        return self.P.add("pe", lambda e: e.transpose(out, in_, ident), r, w)

    def act(self, out, in_, func, r, w, bias=0.0, scale=1.0, eng="act"):
        return self.P.add(eng, lambda e: e.activation(out, in_, func, bias=bias, scale=scale), r, w)

    def copy(self, eng, out, in_, r, w):
        if eng == "act":
            return self.P.add("act", lambda e: e.copy(out, in_), r, w)
        return self.P.add(eng, lambda e: e.tensor_copy(out, in_), r, w)

    def tt(self, eng, out, in0, in1, op, r, w):
        return self.P.add(eng, lambda e: e.tensor_tensor(out, in0, in1, op), r, w)

    def ts(self, eng, out, in0, s1, op0, r, w, s2=None, op1=None):
        if op1 is None:
            return self.P.add(eng, lambda e: e.tensor_scalar(out, in0, s1, None, op0), r, w)
        return self.P.add(eng, lambda e: e.tensor_scalar(out, in0, s1, s2, op0, op1), r, w)

    def stt(self, eng, out, in0, scalar, in1, op0, op1, r, w):
        return self.P.add(eng, lambda e: e.scalar_tensor_tensor(out, in0, scalar, in1, op0, op1), r, w)

    def recip(self, out, in_, r, w):
        return self.P.add("dve", lambda e: e.reciprocal(out, in_), r, w)

    def memset(self, eng, ap, val, w):
        return self.P.add(eng, lambda e: e.memset(ap, val), (), w)


def phase_M(C, G, l, do_layer, do_norm, x_d, x_db, xo_d, xo_db):
    out_x = xo_d is not None
    with contextlib.ExitStack() as st:
        C.stack = st
        id_d = G["ident"]
        if do_layer:
            ot_all = G["OT_all"]
            wo_d, gog_d, mlg_d, wup_d, wdn_d = (G["%s_%d" % (n, l)] for n in ("w_o", "gog", "mlg", "w_up", "w_down"))
        xnT_d = G["xnT_own"]

        X, _ = C.sb([128, 16, D], F32, "X")
        Xb = [Buf("X%d" % i) for i in range(16)]
        ident, identb = C.sb([128, 128], BF16, "ident")
        C.dma("sp", ident[:, :], id_d[:, :], (), (identb,))
        xv = x_d.rearrange("(b p) d -> p b d", p=128)
        for tb in range(16):
            C.dma("sp" if tb % 2 == 0 else "pool", X[:, tb, :], xv[:, tb, :], (x_db,), (Xb[tb],))
        AT, _ = C.sb([128, 8, 2048], BF16, "AT")
        ATb = [Buf("AT%d" % i) for i in range(16)]
        pst = [C.ps(BF16, "pst") for _ in range(2)]
        pstr = Ring(pst)
        psf = Ring([C.ps(F32, "psf") for _ in range(6)])
        sq_r = C.sbring(2, [128, D], F32, "sq")
        xnb_r = C.sbring(2, [128, D], BF16, "xnb")
        st_r = C.sbring(4, [128, 4], F32, "stat")

        def norm_T(tb, dstT, dstb):
            sq, sqb = sq_r.next()
            C.act(sq[:, :], X[:, tb, :], AF.Square, (Xb[tb],), (sqb,))
            s, sb_ = st_r.next()
            C.P.add("dve", lambda e: e.reduce_sum(s[:, 0:1], sq[:, :], AX.X), (sqb,), (sb_,))
            C.act(s[:, 1:2], s[:, 0:1], AF.Sqrt, (sb_,), (sb_,), bias=EPS, scale=1.0 / D)
            C.recip(s[:, 2:3], s[:, 1:2], (sb_,), (sb_,))
            xnb, xnbb = xnb_r.next()
            C.ts("dve", xnb[:, :], X[:, tb, :], s[:, 2:3], ALU.mult, (Xb[tb], sb_), (xnbb,))
            pt, ptb = pstr.next()
            for kc in range(8):
                C.tr(pt[:, kc * 128:(kc + 1) * 128], xnb[:, kc * 128:(kc + 1) * 128], ident[:, :],
                     (xnbb, identb), (ptb,))
            C.copy("act", dstT[:, :, tb * 128:(tb + 1) * 128],
                   pt[:, :].rearrange("p (k t) -> p k t", k=8), (ptb,), dstb if isinstance(dstb, tuple) else (dstb,))

        if do_layer:
            gog, gogb = C.sb([128, 8], F32, "gog")
            mlg, mlgb = C.sb([128, 8], F32, "mlg")
            C.dma("sp", gog[:, :], gog_d[:, :], (), (gogb,))
            C.dma("sp", mlg[:, :], mlg_d[:, :], (), (mlgb,))
            ones, onesb = C.sb([128, 1], BF16, "ones")
            C.memset("pool", ones[:, :], 1.0, (onesb,))
            if True:
                WU, wob = C.sb([128, 8192], BF16, "WU", multi=True)
                wo = WU[:, :].rearrange("p (k n) -> p k n", k=8)
                stg_r = C.sbring(2, [128, D], F32, "wostg")
                wov = wo_d.rearrange("(k p) n -> p k n", p=128)
                for kc in range(8):
                    sg, sgb = stg_r.next()
                    C.dma("sp", sg[:, :], wov[:, kc, :], (), (sgb,))
                    C.P.add("act", lambda e, kc=kc, sg=sg: e.activation(wo[:, kc, :], sg[:, :], AF.Copy, scale=gog[:, kc:kc + 1]),
                            (sgb, gogb), (wob,))
                ATm = [Buf("ATm%d" % m) for m in range(4)]
                for m in (1, 0, 3, 2):
                    otv = ot_all[m].rearrange("(a r) t -> r a t", a=2)
                    for b2 in range(2):
                        def f(e, m=m, b2=b2, otv=otv):
                            off = C.pid(e, lambda p: (p % 4) * 2048, "i4")
                            return e.dma_start(out=AT[b2 * 64:(b2 + 1) * 64, 2 * m:2 * m + 2, :],
                                               in_=otv[b2 * 64:(b2 + 1) * 64, :, bass.ds(off, 2048)])
                        C.P.add("sp", f, (G["OT_all_b"][m],), (ATm[m],), dma=True)
                sqo_r = C.sbring(2, [128, 8, 128], BF16, "sqo")
                for gs in ((0, 1, 3), (2,)):
                    for tb in range(16):
                        tsl = slice(tb * 128, (tb + 1) * 128)
                        sqo, sqob = sqo_r.next()
                        for g in gs:
                            C.tt("pool", sqo[:, 2 * g:2 * g + 2, :], AT[:, 2 * g:2 * g + 2, tsl],
                                 AT[:, 2 * g:2 * g + 2, tsl], ALU.mult, (ATm[g],), (sqob,))
                        pss, pssb = psf.next()
                        for g in gs:
                            for j in range(2):
                                C.mm(pss[:, g:g + 1], sqo[:, 2 * g + j, :], ones[:, :], j == 0, j == 1,
                                     (sqob, onesb), (pssb,))
                        s, sb_ = st_r.next()
                        rs, rsb = st_r.next()
                        cl = slice(0, 4) if len(gs) > 1 else slice(2, 3)
                        C.act(s[:, cl], pss[:, cl], AF.Sqrt, (pssb,), (sb_,), bias=EPS, scale=1.0 / 256)
                        C.recip(rs[:, cl], s[:, cl], (sb_,), (rsb,))
                        for half in range(2):
                            hs = slice(half * 512, (half + 1) * 512)
                            for g in gs:
                                pg, pgb = psf.next()
                                for j in range(2):
                                    C.mm(pg[:, :], AT[:, 2 * g + j, tsl], wo[:, 2 * g + j, hs], j == 0, j == 1,
                                         (ATm[g], wob), (pgb,))
                                C.stt("dve", X[:, tb, hs], pg[:, :], rs[:, g:g + 1], X[:, tb, hs], ALU.mult, ALU.add,
                                      (pgb, rsb, Xb[tb]), (Xb[tb],))
            if True:
                for tb in range(16):
                    norm_T(tb, AT, (ATb[tb],) + tuple(ATm))
                ustg_r = C.sbring(3, [128, 2, 512], F32, "ustg")
                dstg_r = C.sbring(2, [128, 1024], F32, "dstg")
                wup_r = Ring([C.sb([128, 8, 512], BF16, "wup", multi=True) for _ in range(2)])
                wdn_r = Ring([C.sb([128, 4, D], BF16, "wdn", multi=True) for _ in range(2)])
                uT = WU[:, :].rearrange("p (k n) -> p k n", k=4)
                uTb = [[Buf("uT%d_%d" % (a, b)) for b in range(4)] for a in range(4)]
                sq2_r = C.sbring(2, [128, 512], F32, "sq2")
                wupv = wup_d.rearrange("(k p) n -> p k n", p=128)
                wdnv = wdn_d.rearrange("(k p) n -> p k n", p=128)
                for fg in range(8):
                    wup, wupb = wup_r.next()
                    wdn, wdnb = wdn_r.next()
                    for k2 in range(4):
                        sg, sgb = ustg_r.next()
                        C.dma("sp", sg[:, :, :], wupv[:, 2 * k2:2 * k2 + 2, fg * 512:(fg + 1) * 512], (), (sgb,))
                        for j in range(2):
                            kc = 2 * k2 + j
                            C.P.add("act", lambda e, kc=kc, sg=sg, j=j, wup=wup: e.activation(
                                wup[:, kc, :], sg[:, j, :], AF.Copy, scale=mlg[:, kc:kc + 1]), (sgb, mlgb), (wupb,))
                    for fc in range(4):
                        sg, sgb = dstg_r.next()
                        C.dma("pool", sg[:, :], wdnv[:, fg * 4 + fc, :], (), (sgb,))
                        C.copy("act" if fc % 2 else "dve", wdn[:, fc, :], sg[:, :], (sgb,), (wdnb,))
                    for fc in range(4):
                        for tg in range(4):
                            pu, pub = psf.next()
                            for kc in range(8):
                                C.mm(pu[:, :], wup[:, kc, fc * 128:(fc + 1) * 128],
                                     AT[:, kc, tg * 512:(tg + 1) * 512], kc == 0, kc == 7,
                                     (wupb,) + tuple(ATb[4 * tg:4 * tg + 4]), (pub,))
                            sq2, sq2b = sq2_r.next()
                            C.act(sq2[:, :], pu[:, :], AF.Square, (pub,), (sq2b,))
                            C.stt("dve", uT[:, fc, tg * 512:(tg + 1) * 512], pu[:, :], 0.0, sq2[:, :],
                                  ALU.is_gt, ALU.mult, (pub, sq2b), (uTb[fc][tg], wob))
                    for tb in range(16):
                        tsl = slice(tb * 128, (tb + 1) * 128)
                        for half in range(2):
                            hs = slice(half * 512, (half + 1) * 512)
                            pd, pdb = psf.next()
                            for fc in range(4):
                                C.mm(pd[:, :], uT[:, fc, tsl], wdn[:, fc, hs], fc == 0, fc == 3,
                                     (uTb[fc][tb // 4], wdnb), (pdb,))
                            C.tt("dve", X[:, tb, hs], pd[:, :], X[:, tb, hs], ALU.add, (pdb, Xb[tb]), (Xb[tb],))
        if out_x:
            xov = xo_d.rearrange("(b p) d -> p b d", p=128)
            for tb in range(16):
                C.dma("sp" if tb % 2 == 0 else "pool", xov[:, tb, :], X[:, tb, :], (Xb[tb],), (xo_db,))
        if do_norm:
            for q in range(4):
                for tb in range(4 * q, 4 * q + 4):
                    norm_T(tb, AT, ATb[tb])
                xnv = xnT_d[q].rearrange("(k p) t -> p k t", p=128)
                C.dma("sp", xnv, AT[:, :, q * 512:(q + 1) * 512],
                      tuple(ATb[4 * q:4 * q + 4]), (G["xnT_own_b"][q],))
                allgather(C, G["xnT_own_t"][q], G["xnT_own_b"][q], G["xnT_all_t"][q], G["xnT_all_b"][q])
        C.P.emit()


def allgather(C, src_t, src_b, dst_t, dst_b):
    C.P.add("pool", lambda e: e.collective_compute(
        "AllGather", ALU.bypass, replica_groups=[[0, 1, 2, 3], [4, 5, 6, 7]],
        ins=[src_t.ap().opt()], outs=[dst_t.ap().opt()]), (src_b,), (dst_b,), dma=True, cc=True)


NAB = 640
NCD = 384


A_LAYER_INPUTS = (("ang", [128, 8], F32), ("wAB", [D, NAB], F32), ("wCD", [D, NCD], F32), ("wuq", [256, 192], F32),
                  ("gql", [128, 2], F32), ("wukv", [128, 128], F32), ("gkvl", [128, 1], F32), ("gcol", [128, 8], F32),
                  ("sink", [128, 1], F32))
A_SHARED_INPUTS = (("rope", [64, S], F32), ("T2", [128, 128], F32), ("PM", [128, 128], F32), ("kbias", [128, 2], F32),
                   ("OH", [32, S], BF16), ("cst", [128, 512], BF16), ("Wt", [128, 1024], BF16), ("SEL", [128, 64], F32))


def phase_A(C, G, l):
    with contextlib.ExitStack() as st:
        C.stack = st
        (ang_d, wAB_d, wCD_d, wuq_d, gql_d, wukv_d, gkvl_d, gcol_d, esk_d) = (G["%s_%d" % (n, l)] for n, _, _ in A_LAYER_INPUTS)
        (rope_d, T2_d, PM_d, kb_d, OH_d, cst_d, W_d, sel_d) = (G[n] for n, _, _ in A_SHARED_INPUTS)
        xnT_all, xnT_all_b = G["xnT_all"], G["xnT_all_b"]
        OT_d, OT_b = G["OT_own"], G["OT_own_b"]

        ld = lambda shape, dt, src, nm, eng="sp": _load(C, shape, dt, src, nm, eng)
        ang, angb = ld([128, 8], F32, ang_d[:, :], "ang")
        gql, gqlb = ld([128, 2], F32, gql_d[:, :], "gql")
        gkvl, gkvlb = ld([128, 1], F32, gkvl_d[:, :], "gkvl")
        gcol, gcolb = ld([128, 8], F32, gcol_d[:, :], "gcol")
        T2, T2b = ld([128, 128], F32, T2_d[:, :], "T2")
        PM, PMb = ld([128, 128], F32, PM_d[:, :], "PM")
        kbias, kbb = ld([128, 2], F32, kb_d[:, :], "kbias")
        cst, cstb = ld([128, 512], BF16, cst_d[:, :], "cst")
        Wt, Wtb = ld([128, 1024], BF16, W_d[:, :], "Wt")
        SEL, SELb = ld([128, 64], F32, sel_d[:, :], "SEL")
        esk, eskb = ld([128, 1], F32, esk_d[:, :], "esk")
        C.act(esk[:, :], esk[:, :], AF.Exp, (eskb,), (eskb,))
        epsc, epsb = C.sb([128, 1], F32, "epsc")
        C.memset("pool", epsc[:, :], EPS, (epsb,))
        ident = cst[:, 0:128]
        TRI = cst[:, 128:256]
        BD = cst[:, 256:384]
        ONES = cst[:, 384:512]

        T1, T1b = C.sb([128, S], BF16, "T1", multi=True)
        T2t, T2tb = C.sb([128, S], BF16, "T2t", multi=True)
        T3, T3b = C.sb([128, S], BF16, "T3", multi=True)
        T4, T4b = C.sb([128, S], BF16, "T4", multi=True)
        Vt = [C.sb([128, 64, 65], BF16, "V%d" % i, multi=True) for i in range(4)]
        for (v, vb) in Vt:
            C.memset("pool", v[:, :, :], 1.0, (vb,))
        RAWOCT, _ = C.sb([128, 6 * 512], F32, "RAWOCT")
        OCT, OCTb = RAWOCT[:, 0:2048], Buf("OCT")
        raw_items = [(RAWOCT[:, i * 512:(i + 1) * 512], Buf("raw%d" % i)) for i in range(6)]

        wAB, wABb = C.sb([128, 8, NAB], BF16, "wAB", multi=True)
        wCD, wCDb = C.sb([128, 8, NCD], BF16, "wCD", multi=True)
        wuq, wuqb = C.sb([128, 2, 192], BF16, "wuq", multi=True)
        wukv, wukvb = C.sb([128, 128], BF16, "wukv")
        wABv = wAB_d.rearrange("(k p) n -> p k n", p=128)
        wCDv = wCD_d.rearrange("(k p) n -> p k n", p=128)
        wuqv = wuq_d.rearrange("(k p) n -> p k n", p=128)
        wload = []

        def stage_w(dst, dstb, src, ncols, scol, scolb, nk, q):
            for kc in range(nk):
                for c0 in range(0, ncols, 512):
                    n = min(512, ncols - c0)
                    sg, sgb = stg_ring.next()
                    sv = src[:, kc, c0:c0 + n] if nk > 1 or len(src.shape) == 3 else src[:, c0:c0 + n]
                    dv = dst[:, kc, c0:c0 + n] if len(dst.shape) == 3 else dst[:, c0:c0 + n]
                    C.dma(q, sg[:, 0:n], sv, (), (sgb,))
                    C.ts("dve", dv, sg[:, 0:n], scol[:, kc:kc + 1], ALU.mult, (sgb, scolb), (dstb,))

        psA = Ring([C.ps(F32, "psA") for _ in range(3)])
        psB = Ring([C.ps(F32, "psB") for _ in range(2)])
        psC = Ring([C.ps(F32, "psC") for _ in range(2)])
        psT, psTb = C.ps(BF16, "psT")
        xn_r = C.sbring(2, [128, 8, 512], BF16, "xn")
        f_r = C.sbring(6, [128, 512], F32, "f")
        qr_r = C.sbring(4, [128, 512], F32, "qr")
        vt_r = C.sbring(4, [128, 512], BF16, "vt")
        h_r = C.sbring(4, [128, 512], BF16, "h")
        rp_r = C.sbring(2, [128, 2, 512], F32, "rp")
        xnv4 = [a.rearrange("(r k p) t -> r p k t", r=4, p=128) for a in xnT_all]

        def load_xn(c):
            xn, xnb = xn_r.next()
            src = xnv4[c % 4][c // 4]
            C.dma("sp", xn[:, 0:4, :], src[:, 0:4, :], (xnT_all_b[c % 4],), (xnb,))
            C.dma("pool", xn[:, 4:8, :], src[:, 4:8, :], (xnT_all_b[c % 4],), (xnb,))
            return xn, xnb
        stg_ring = Ring(f_r.items + raw_items)
        stage_w(wAB, wABb, wABv, NAB, ang, angb, 8, "act")
        stage_w(wuq, wuqb, wuqv, 192, gql, gqlb, 2, "act")
        stage_w(wukv, wukvb, wukv_d, 128, gkvl, gkvlb, 1, "act")
        ksum, ksumb = C.sb([64, 32], F32, "ksum")
        C_qlb = C.sbring(4, [128, 2, 512], BF16, "ql")

        def proj(xn, xnb, c0, ncol):
            p, pb = psA.next()
            for kc in range(8):
                C.mm(p[0:ncol, :], wsrc[0][:, kc, c0:c0 + ncol], xn[:, kc, :], kc == 0, kc == 7,
                     (wsrc[1], xnb), (pb,))
            return p, pb

        def rstd_of(src, srcb, lo, hi, ones_ap, n, extra=None):
            h, hb = h_r.next()
            C.act(h[lo:hi, :], src[lo:hi, :], AF.Square, (srcb,), (hb,))
            p, pb = psB.next()
            C.mm(p[lo:hi, :], ones_ap, h[lo:hi, :], True, True, (hb, cstb), (pb,))
            f, fb = f_r.next()
            C.act(f[lo:hi, :], p[lo:hi, :], AF.Ln, (pb, epsb), (fb,), bias=epsc[lo:hi, 0:1], scale=1.0 / n)
            C.act(f[lo:hi, :], f[lo:hi, :], AF.Exp, (fb,), (fb,), scale=-0.5)
            return f, fb

        def vtrans(vT, vTb, base, tok0, stride, V, Vb, blk0, nblk):
            for j in range(nblk):
                a = tok0 + j * 128 * stride
                C.tr(psT[:, j * 64:(j + 1) * 64], vT[base:base + 64, a:a + 127 * stride + 1:stride],
                     cst[base:base + 64, base:base + 64], (vTb, cstb), (psTb,))
            C.copy("act", V[:, blk0:blk0 + nblk, 0:64],
                   psT[:, 0:nblk * 64].rearrange("p (j d) -> p j d", j=nblk), (psTb,), (Vb,))

        QA, KA, QB, KB = T1, T2t, T3, T4
        VA, VAb = Vt[0]
        VB, VBb = Vt[1]
        C.dma("sp", KA[64:96, :], OH_d[:, :], (), (T2tb,))
        wsrc = [wAB, wABb]

        raw_r = Ring(raw_items)
        hk_r = C.sbring(4, [128, 512], BF16, "hk")

        def stat(h_ap, hb, ones_ap, lo, hi, n, k2=None):
            p, pb = psB.next()
            if k2 is None:
                C.mm(p[lo:hi, :], ones_ap, h_ap, True, True, (hb, cstb), (pb,))
            else:
                for j in range(2):
                    C.mm(p[lo:hi, :], ones_ap, k2[:, j, :], j == 0, j == 1, (hb, cstb), (pb,))
            f, fb = f_r.next()
            C.act(f[lo:hi, :], p[lo:hi, :], AF.Ln, (pb, epsb), (fb,), bias=epsc[lo:hi, 0:1], scale=1.0 / n)
            C.act(f[lo:hi, :], f[lo:hi, :], AF.Exp, (fb,), (fb,), scale=-0.5)
            return f, fb

        def stageA(c, mid=None):
            cs = slice(c * 512, (c + 1) * 512)
            xn, xnb = load_xn(c)
            rp, rpb = rp_r.next()
            C.dma("sp", rp[64:96, 0, :], rope_d[0:32, cs], (), (rpb,))
            C.dma("sp", rp[64:96, 1, :], rope_d[32:64, cs], (), (rpb,))
            raws = []
            for (c0, n) in ((0, 96), (96, 64), (160, 96)):
                p, pb = proj(xn, xnb, c0, n)
                rw, rwb = raw_r.next()
                C.copy("act", rw[0:n, :], p[0:n, :], (pb,), (rwb,))
                raws.append((rw, rwb))
            if mid is not None:
                mid()
            qlb, qlbb = C_qlb.next()
            hq, hqb = C_qlb.next()
            for j in range(2):
                p, pb = proj(xn, xnb, 256 + 128 * j, 128)
                C.copy("act", qlb[:, j, :], p[:, :], (pb,), (qlbb,))
                C.tt("dve", hq[:, j, :], p[:, :], qlb[:, j, :], ALU.mult, (pb, qlbb), (hqb,))
            p5, p5b = proj(xn, xnb, 512, 128)
            kvb, kvbb = hk_r.next()
            hkv, hkvb = hk_r.next()
            C.copy("act", kvb[:, :], p5[:, :], (p5b,), (kvbb,))
            C.tt("dve", hkv[:, :], p5[:, :], kvb[:, :], ALU.mult, (p5b, kvbb), (hkvb,))
            (r3, r3b) = raws[2]
            rl, rlb = stat(None, hqb, ONES, 0, 128, 256, k2=hq)
            rkv, rkvb = stat(hkv[:, :], hkvb, ONES, 0, 128, 128)
            P1, P1b = psC.next()
            for j in range(2):
                C.mm(P1[0:96, :], wuq[:, j, 0:96], qlb[:, j, :], j == 0, j == 1, (wuqb, qlbb), (P1b,))
            QR, QRb = qr_r.next()
            C.tt("dve", QR[0:96, :], P1[0:96, :], rl[0:96, :], ALU.mult, (P1b, rlb), (QRb,))
            P2, P2b = psC.next()
            for j in range(2):
                C.mm(P2[0:96, :], wuq[:, j, 96:192], qlb[:, j, :], j == 0, j == 1, (wuqb, qlbb), (P2b,))
            QRP, QRPb = qr_r.next()
            C.tt("dve", QRP[64:96, :], P2[64:96, :], rl[64:96, :], ALU.mult, (P2b, rlb), (QRPb,))
            vT, vTb = vt_r.next()
            C.copy("pool", vT[0:64, :], r3[0:64, :], (r3b,), (vTb,))
            Pk, Pkb = psC.next()
            C.mm(Pk[0:64, :], wukv[:, 0:64], kvb[:, :], True, True, (wukvb, kvbb), (Pkb,))
            C.tt("dve", r3[0:64, :], Pk[0:64, :], rkv[0:64, :], ALU.mult, (Pkb, rkvb, vTb), (r3b,))
            Pv, Pvb = psC.next()
            C.mm(Pv[0:64, :], wukv[:, 64:128], kvb[:, :], True, True, (wukvb, kvbb), (Pvb,))
            vT2, vT2b = vt_r.next()
            C.tt("dve", vT2[0:64, :], Pv[0:64, :], rkv[0:64, :], ALU.mult, (Pvb, rkvb), (vT2b,))
            return dict(c=c, rp=(rp, rpb), raws=raws, QR=(QR, QRb), QRP=(QRP, QRPb), vT=(vT, vTb), vT2=(vT2, vT2b))

        def stageB(S_):
            c = S_["c"]
            cs = slice(c * 512, (c + 1) * 512)
            rp, rpb = S_["rp"]
            (r1, r1b), (r2, r2b), (r3, r3b) = S_["raws"]
            QR, QRb = S_["QR"]
            QRP, QRPb = S_["QRP"]
            vT, vTb = S_["vT"]
            vT2, vT2b = S_["vT2"]
            vtrans(vT, vTb, 0, 0, 1, VA, VAb, 4 * c, 4)
            vtrans(vT2, vT2b, 0, 0, 1, VB, VBb, 4 * c, 4)
            hs = []
            for (src, srcb, lo, hi) in ((r1, r1b, 0, 64), (r2, r2b, 0, 64), (QR, QRb, 0, 96), (r3, r3b, 0, 96)):
                h, hb = h_r.next()
                C.act(h[lo:hi, :], src[lo:hi, :], AF.Square, (srcb,), (hb,))
                hs.append((h, hb))
            rsq, rsqb = stat(hs[0][0][0:64, :], hs[0][1], ONES[0:64, 0:64], 0, 64, 64)
            rsk, rskb = stat(hs[1][0][0:64, :], hs[1][1], ONES[0:64, 0:64], 0, 64, 64)
            rq, rqb = stat(hs[2][0][0:96, :], hs[2][1], ONES[0:96, 0:96], 0, 96, 96)
            rk, rkb = stat(hs[3][0][0:96, :], hs[3][1], ONES[0:96, 0:96], 0, 96, 96)
            C.stt("dve", QA[0:64, cs], r1[0:64, :], gcol[0:64, 0:1], rsq[0:64, :], ALU.mult, ALU.mult,
                  (r1b, gcolb, rsqb), (T1b,))
            C.stt("dve", r2[0:64, :], r2[0:64, :], gcol[0:64, 1:2], rsk[0:64, :], ALU.mult, ALU.mult,
                  (r2b, gcolb, rskb), (r2b,))
            C.copy("pool", KA[0:64, cs], r2[0:64, :], (r2b,), (T2tb,))
            C.P.add("dve", lambda e, r2=r2, c=c: e.reduce_sum(
                ksum[0:64, 2 * c:2 * c + 2], r2[0:64, :].rearrange("p (a b) -> p a b", a=2), AX.X),
                (r2b,), (ksumb,))
            C.stt("dve", QB[0:64, cs], QR[0:64, :], gcol[0:64, 2:3], rq[0:64, :], ALU.mult, ALU.mult,
                  (QRb, gcolb, rqb), (T3b,))
            C.stt("dve", QR[64:96, :], QR[64:96, :], gcol[64:96, 2:3], rp[64:96, 0, :], ALU.mult, ALU.mult,
                  (QRb, gcolb, rpb), (QRb,))
            C.stt("dve", QRP[64:96, :], QRP[64:96, :], gcol[64:96, 3:4], rp[64:96, 1, :], ALU.mult, ALU.mult,
                  (QRPb, gcolb, rpb), (QRPb,))
            C.tt("pool", QR[64:96, :], QR[64:96, :], QRP[64:96, :], ALU.add, (QRb, QRPb), (QRb,))
            C.tt("pool", QB[64:96, cs], QR[64:96, :], rq[64:96, :], ALU.mult, (QRb, rqb), (T3b,))
            C.stt("dve", KB[0:64, cs], r3[0:64, :], gcol[0:64, 4:5], rk[0:64, :], ALU.mult, ALU.mult,
                  (r3b, gcolb, rkb), (T4b,))
            C.stt("dve", r3[64:96, :], r3[64:96, :], gcol[64:96, 4:5], rp[64:96, 0, :], ALU.mult, ALU.mult,
                  (r3b, gcolb, rpb), (r3b,))
            C.stt("dve", r1[64:96, :], r1[64:96, :], gcol[64:96, 5:6], rp[64:96, 1, :], ALU.mult, ALU.mult,
                  (r1b, gcolb, rpb), (r1b,))
            C.tt("pool", r3[64:96, :], r3[64:96, :], r1[64:96, :], ALU.add, (r3b, r1b), (r3b,))
            C.tt("pool", KB[64:96, cs], r3[64:96, :], rk[64:96, :], ALU.mult, (r3b, rkb), (T4b,))

        prevS = None
        for c in [r * 4 + j for j in range(4) for r in range(4)]:
            curS = stageA(c, (lambda p=prevS: stageB(p)) if prevS is not None else None)
            prevS = curS
        stageB(prevS)

        stg_ring = Ring(f_r.items)
        stage_w(wCD, wCDb, wCDv, NCD, ang, angb, 8, "sp")

        E_r = C.sbring(4, [128, 512], BF16, "E")
        Osb_r = C.sbring(2, [128, 512], F32, "Osb")
        oo_r = C.sbring(2, [64, 512], BF16, "oo")

        def finalize(Osrc, Osrcb, m, col0, sink=False):
            pd, pdb = psC.next()
            C.mm(pd[0:64, :], SEL[0:65, 0:64], Osrc, True, True, (Osrcb, SELb), (pdb,))
            rc, rcb = f_r.next()
            if sink:
                C.ts("dve", rc[0:64, :], pd[0:64, :], esk[0:64, 0:1], ALU.add, (pdb, eskb), (rcb,))
                C.recip(rc[0:64, :], rc[0:64, :], (rcb,), (rcb,))
            else:
                C.recip(rc[0:64, :], pd[0:64, :], (pdb,), (rcb,))
            oo, oob = oo_r.next()
            C.tt("dve", oo[0:64, :], Osrc[0:64, :], rc[0:64, :], ALU.mult, (Osrcb, rcb), (oob,))
            C.dma("sp", OT_d[m][:, col0:col0 + 512], oo[0:64, :], (oob,), (OT_b[m],))

        class Task:
            __slots__ = ("qk", "ex", "pv", "newO", "fin", "after")

        def run_tasks(tasks, look=2):
            n = len(tasks)
            Sx = [None] * n

            def emit_qk(i):
                Sx[i] = psA.next()
                tasks[i].qk(*Sx[i])
            for i in range(min(look, n)):
                emit_qk(i)
            pend = []
            O = None
            for i, t in enumerate(tasks):
                if i + look < n:
                    emit_qk(i + look)
                E = E_r.next()
                t.ex(Sx[i][0], Sx[i][1], E[0], E[1])
                if t.newO:
                    O = psB.next()
                t.pv(E[0], E[1], O[0], O[1])
                for f in pend:
                    f()
                pend = []
                if t.fin is not None:
                    pend.append(lambda t=t, O=O: t.fin(O[0], O[1]))
                for f in (t.after or ()):
                    f()
            for f in pend:
                f()

        def dense_tasks(Kt, Ktb, Qt, Qtb, rows, V, Vb, scale, bias_of, m):
            tasks = []
            for qt in range(16):
                nkt = 4 * qt + 4
                for kt in range(nkt):
                    j = kt - 4 * qt
                    c0 = 128 * j if j > 0 else 0
                    t = Task()

                    def qk(Sx, Sb, kt=kt, qt=qt, c0=c0):
                        C.mm(Sx[:, c0:512], Kt[0:rows, kt * 128:(kt + 1) * 128],
                             Qt[0:rows, qt * 512 + c0:(qt + 1) * 512], True, True, (Ktb, Qtb), (Sb,))

                    def ex(Sx, Sb, E, Eb, kt=kt, c0=c0, j=j):
                        b = bias_of(kt)
                        rd = (Sb,) if isinstance(b, float) else (Sb, kbb)
                        C.act(E[:, c0:512], Sx[:, c0:512], AF.Exp, rd, (Eb,), bias=b, scale=scale)
                        if j >= 0:
                            C.tt("dve", E[:, c0:c0 + 128], E[:, c0:c0 + 128], TRI, ALU.mult, (Eb, cstb), (Eb,))

                    def pv(E, Eb, O, Ob, kt=kt, c0=c0, nkt=nkt):
                        C.mm(O[0:65, c0:512], V[:, kt, :], E[:, c0:512], kt == 0, kt == nkt - 1, (Vb, Eb), (Ob,))

                    def fin(O, Ob, qt=qt):
                        Osb, Osbb = Osb_r.next()
                        C.copy("act", Osb[0:65, :], O[0:65, :], (Ob,), (Osbb,))
                        finalize(Osb[0:65, :], Osbb, m, qt * 512)
                    t.qk, t.ex, t.pv = qk, ex, pv
                    t.newO = kt == 0
                    t.fin = fin if kt == nkt - 1 else None
                    t.after = None
                    tasks.append(t)
            return tasks

        km, kmb = C.sb([64, 32], BF16, "km")
        C.ts("dve", km[:, :], ksum[:, :], 1.0 / 256, ALU.mult, (ksumb,), (kmb,))
        STg_r = C.sbring(3, [128, 128], BF16, "STg")
        for (stg_, stgb_) in STg_r.items:
            C.memset("pool", stg_[:, :], 0.0, (stgb_,))
        g_r = C.sbring(4, [128, 80], F32, "g")

        def gphase1(qb):
            qblk = qb // 2
            qs = slice(qb * 128, (qb + 1) * 128)
            pg, pgb = psC.next()
            C.mm(pg[:, 0:32], QA[0:64, qs], km[0:64, :], True, True, (T1b, kmb), (pgb,))
            g, gb = g_r.next()
            C.tt("dve", g[:, 0:32], pg[:, 0:32], PM[:, 32 - qblk:64 - qblk], ALU.add, (pgb, PMb), (gb,))
            C.P.add("dve", lambda e: e.max(g[:, 64:72], g[:, 0:32]), (gb,), (gb,))
            C.ts("dve", g[:, 72:73], g[:, 66:67], -1e29, ALU.max, (gb,), (gb,))
            C.ts("dve", g[:, 32:64], g[:, 0:32], g[:, 72:73], ALU.is_ge, (gb,), (gb,))
            C.tt("dve", g[:, 32:64], g[:, 32:64], PM[:, 64 + 32 - qblk:64 + 64 - qblk], ALU.add, (gb, PMb), (gb,))
            C.tt("dve", g[:, 0:32], g[:, 32:64], T2[:, 63 - qb:63 - qb + 64:2], ALU.mult, (gb, T2b), (gb,))
            stg, stgb = STg_r.next()
            C.ts("dve", stg[:, 64:96], g[:, 0:32], -BIG, ALU.add, (gb,), (stgb,))

            def part2():
                C.tr(psT[:, 0:128], stg[:, :], ident, (stgb, cstb), (psTb,))
                C.copy("act", QA[64:96, qs], psT[64:96, 0:128], (psTb,), (T1b,))
            return part2

        def ag_m(m):
            allgather(C, G["OT_own_t"][m], OT_b[m], G["OT_all_t"][m], G["OT_all_b"][m])

        sB = 96 ** -0.5
        tB = dense_tasks(KB, T4b, QB, T3b, 96, VB, VBb, sB, lambda kt: 0.0, 1)
        step = len(tB) // 64
        for qb in range(64):
            i1 = qb * step
            i2 = min(i1 + 5, len(tB) - 1)
            holder = {}

            def f1(qb=qb, holder=holder):
                holder["p2"] = gphase1(qb)

            def f2(holder=holder):
                holder["p2"]()
            tB[i1].after = (tB[i1].after or ()) + (f1,)
            tB[i2].after = (tB[i2].after or ()) + (f2,)
        run_tasks(tB)
        ag_m(1)
        run_tasks(dense_tasks(KA, T2tb, QA, T1b, 96, VA, VAb, 0.125, lambda kt: kbias[:, kt % 2:kt % 2 + 1], 0))
        ag_m(0)

        QCD, KCD, VCDT = T1, T2t, T3
        wsrc[0], wsrc[1] = wCD, wCDb
        VC1, VC1b = Vt[0]
        VC4, VC4b = Vt[1]
        VC16, VC16b = Vt[2]
        VD, VDb = Vt[3]
        for c in range(16):
            cs = slice(c * 512, (c + 1) * 512)
            xn, xnb = load_xn(c)
            for gi, (dst, dstb) in enumerate(((QCD, T1b), (KCD, T2tb))):
                p, pb = proj(xn, xnb, 128 * gi, 128)
                rs, rsb = rstd_of(p, pb, 0, 128, BD, 64)
                C.stt("dve", dst[:, cs], p[:, :], gcol[:, 6 + gi:7 + gi], rs[:, :], ALU.mult, ALU.mult,
                      (pb, gcolb, rsb), (dstb,))
            p, pb = proj(xn, xnb, 256, 128)
            C.copy("act", VCDT[:, cs], p[:, :], (pb,), (T3b,))
            vtrans(VCDT, T3b, 0, c * 512, 1, VC1, VC1b, 4 * c, 4)
            vtrans(VCDT, T3b, 64, c * 512, 1, VD, VDb, 4 * c, 4)
            for r in range(4):
                vtrans(VCDT, T3b, 0, c * 512 + r, 4, VC4, VC4b, r * 16 + c, 1)
            if c % 4 == 3:
                s_ = c // 4
                for r in range(16):
                    vtrans(VCDT, T3b, 0, s_ * 2048 + r, 16, VC16, VC16b, r * 4 + s_, 1)

        def local(base, d, r, nb, n0, nblk, V, Vb, vidx, Wc, fin):
            tasks = []
            kts = list(range(max(n0 - 1, 0), n0 + nblk))
            for kt in kts:
                qlo, qhi = max(kt, n0), min(kt + 1, n0 + nblk - 1)
                ncol = (qhi - qlo + 1) * 128
                woff = 0 if qlo == kt else 128
                ka = kt * 128 * d + r
                qa = qlo * 128 * d + r
                t = Task()

                def qk(Sx, Sb, ka=ka, qa=qa, ncol=ncol):
                    C.mm(Sx[:, 0:ncol], KCD[base:base + 64, ka:ka + 127 * d + 1:d],
                         QCD[base:base + 64, qa:qa + (ncol - 1) * d + 1:d], True, True, (T2tb, T1b), (Sb,))

                def ex(Sx, Sb, E, Eb, ncol=ncol, woff=woff):
                    C.act(E[:, 0:ncol], Sx[:, 0:ncol], AF.Exp, (Sb,), (Eb,), scale=0.125)
                    C.tt("dve", E[:, 0:ncol], E[:, 0:ncol], Wt[:, Wc + woff:Wc + woff + ncol], ALU.mult,
                         (Eb, Wtb), (Eb,))

                def pv(E, Eb, O, Ob, kt=kt, qlo=qlo, qhi=qhi):
                    for qb in range(qlo, qhi + 1):
                        first = (kt == kts[0]) and (qb == qlo)
                        last = (kt == kts[-1]) and (qb == qhi)
                        C.mm(O[0:65, (qb - n0) * 128:(qb - n0 + 1) * 128], V[:, vidx(kt), :],
                             E[:, (qb - qlo) * 128:(qb - qlo + 1) * 128], first, last, (Vb, Eb), (Ob,))
                t.qk, t.ex, t.pv = qk, ex, pv
                t.newO = kt == kts[0]
                t.fin = fin if kt == kts[-1] else None
                t.after = None
                tasks.append(t)
            return tasks

        tD = []
        for qt in range(16):
            def finD(O, Ob, qt=qt):
                Osb, Osbb = Osb_r.next()
                C.copy("act", Osb[0:65, :], O[0:65, :], (Ob,), (Osbb,))
                finalize(Osb[0:65, :], Osbb, 3, qt * 512, sink=True)
            tD += local(64, 1, 0, 64, 4 * qt, 4, VD, VDb, lambda kt: kt, 768, finD)
        run_tasks(tD)
        ag_m(3)

        for s_ in range(4):
            tC = []

            def acc(O, Ob, off, d, ncol):
                dst = OCT[0:65, off:off + (ncol - 1) * d + 1:d]
                C.tt("dve", dst, O[0:65, 0:ncol], dst, ALU.add, (Ob, OCTb), (OCTb,))
            for q4 in range(4):
                def fin1(O, Ob, q4=q4):
                    C.copy("act", OCT[0:65, q4 * 512:(q4 + 1) * 512], O[0:65, :], (Ob,),
                           (OCTb,) + tuple(rb for _, rb in raw_items))
                tC += local(0, 1, 0, 64, 16 * s_ + 4 * q4, 4, VC1, VC1b, lambda kt: kt, 0, fin1)
            for r in range(4):
                tC += local(0, 4, r, 16, 4 * s_, 4, VC4, VC4b, lambda kt, r=r: r * 16 + kt, 256,
                            lambda O, Ob, r=r: acc(O, Ob, r, 4, 512))
            for r in range(16):
                tC += local(0, 16, r, 4, s_, 1, VC16, VC16b, lambda kt, r=r: r * 4 + kt, 512,
                            lambda O, Ob, r=r: acc(O, Ob, r, 16, 128))
            run_tasks(tC)
            for q4 in range(4):
                finalize(OCT[0:65, q4 * 512:(q4 + 1) * 512], OCTb, 2, s_ * 2048 + q4 * 512)
        ag_m(2)
        C.P.emit()


def _load(C, shape, dt, src, nm, eng="sp"):
    t, b = C.sb(shape, dt, nm)
    C.dma(eng, t[tuple(slice(None) for _ in shape)], src, (), (b,))
    return t, b


M_LAYER_INPUTS = (("w_o", [D, D], F32), ("gog", [128, 8], F32), ("mlg", [128, 8], F32),
                  ("w_up", [D, 4096], F32), ("w_down", [4096, D], F32))


_NEEDED = set()


def build_fused(nphase=5):
    nc = bass.Bass("TRN2", target_bir_lowering=False)
    with contextlib.ExitStack() as st:
        C = Ctx(nc, st)
        G = {}
        x_d = C.dram("x", [2048, D], F32, "ExternalInput")
        G["ident"] = C.dram("ident", [128, 128], BF16, "ExternalInput")
        for l in range(2):
            if nphase >= 3 + 2 * l:
                for n, shp, dt in M_LAYER_INPUTS:
                    G["%s_%d" % (n, l)] = C.dram("%s_%d" % (n, l), shp, dt, "ExternalInput")
            if nphase >= 2 + 2 * l:
                for n, shp, dt in A_LAYER_INPUTS:
                    G["%s_%d" % (n, l)] = C.dram("%s_%d" % (n, l), shp, dt, "ExternalInput")
        if nphase >= 2:
            for n, shp, dt in A_SHARED_INPUTS:
                G[n] = C.dram(n, shp, dt, "ExternalInput")
        _NEEDED.clear()
        _NEEDED.update(k for k in G)
        _NEEDED.update(("x", "ident"))
        xo_d = C.dram("xo", [2048, D], F32, "ExternalOutput")
        t = nc.dram_tensor("xres", [2048, D], F32)
        G["xres"], G["xres_b"] = t.ap(), Buf("xres", multi=True)
        for n, shp in (("xnT_own", [D, 512]), ("xnT_all", [4 * D, 512]), ("OT_own", [64, S]), ("OT_all", [256, S])):
            ts_ = [nc.dram_tensor("%s%d" % (n, j), list(shp), BF16) for j in range(4)]
            G[n + "_t"] = ts_
            G[n] = [t.ap() for t in ts_]
            G[n + "_b"] = [Buf("%s%d" % (n, j), multi=True) for j in range(4)]
        xin_b, xo_b = Buf("xin"), Buf("xo", multi=True)
        phase_M(C, G, 0, False, True, x_d, xin_b, None, None)
        if nphase >= 2:
            phase_A(C, G, 0)
        if nphase >= 3:
            phase_M(C, G, 0, True, True, x_d, xin_b, G["xres"], G["xres_b"])
        if nphase >= 4:
            phase_A(C, G, 1)
        if nphase >= 5:
            phase_M(C, G, 1, True, False, G["xres"], G["xres_b"], xo_d, xo_b)
        else:
            C.stack = st
            C.dma("sp", xo_d[0:128, :], x_d[0:128, :], (), (xo_b,))
            C.P.emit()
    return nc


_CACHE = {}


def _prog(key, fn):
    if key not in _CACHE:
        _CACHE[key] = fn()
    return _CACHE[key]


def _consts(i):
    f8 = np.float64
    sl_a = 2.0 ** (-8.0 * (3 * i + 1) / 12)
    sl_c = 2.0 ** (-8.0 * (3 * i + 2) / 12)
    sl_d = 2.0 ** (-8.0 * (3 * i + 3) / 12)
    p = np.arange(128, dtype=f8)[:, None]
    j = np.arange(128, dtype=f8)[None, :]
    T2 = (-8.0 * sl_a * (128.0 * (63 - j) + p - 128.0) + BIG).astype(np.float32)
    PM = np.zeros((128, 128), np.float32)
    PM[:, 32:64] = -1e30
    PM[:, 64 + 32] = 1.0
    kbias = np.stack([sl_a * (par * 128 + np.arange(128, dtype=f8) - 128) for par in (0, 1)], 1).astype(np.float32)
    k = np.arange(128)[:, None]
    q = np.arange(128)[None, :]
    cst = np.zeros((128, 512), np.float32)
    cst[:, 0:128] = np.eye(128)
    cst[:, 128:256] = (k <= q)
    cst[:, 256:384] = (k // 64 == q // 64)
    cst[:, 384:512] = 1.0
    Wt = np.zeros((128, 1024), f8)
    for bi, d in enumerate((1, 4, 16)):
        Wt[:, bi * 256:bi * 256 + 128] = (k <= q) * np.exp(-sl_c * d * np.maximum(q - k, 0))
        Wt[:, bi * 256 + 128:bi * 256 + 256] = (k >= q) * np.exp(-sl_c * d * (128 + q - k))
    Wt[:, 768:896] = (k <= q) * np.exp(-sl_d * np.maximum(q - k, 0))
    Wt[:, 896:1024] = (k > q) * np.exp(-sl_d * (128 + q - k))
    SEL = np.zeros((128, 64), np.float32)
    SEL[64, :] = 1.0
    return dict(T2=T2, PM=PM, kbias=kbias, cst=cst.astype(NPBF), Wt=Wt.astype(np.float32).astype(NPBF), SEL=SEL)


def _shared_consts():
    inv = 1.0 / (10000.0 ** (np.arange(0, 32, 2, dtype=np.float32) / 32))
    ang = np.arange(S, dtype=np.float32)[:, None] * inv[None, :]
    cos, sin = np.cos(ang).T, np.sin(ang).T
    rope = np.concatenate([cos, cos, -sin, sin], 0).astype(np.float32)
    OH = (np.arange(S)[None, :] // 256 == np.arange(32)[:, None]).astype(np.float32).astype(NPBF)
    return np.ascontiguousarray(rope), np.ascontiguousarray(OH)


def _col(v, n=128):
    o = np.zeros((128,), np.float32)
    o[:len(v)] = v
    return o


def _attn_inputs(P, l, i, xnT_b, rope, OH):
    w_in = P["w_in"][l]
    kv = i // 2
    sel = lambda a, n=64: list(range(a, a + n))
    krp = list(range(1152 + 16, 1184)) + list(range(1152, 1168))
    colsAB = (sel(64 * i) + krp + sel(256 + 64 * i) + sel(512 + 64 * i) + sel(1152, 32)
              + sel(768, 256) + sel(1024, 128))
    colsCD = (sel(1184 + 64 * i) + sel(1952 + 64 * i) + sel(1440 + 64 * i) + sel(2208 + 64 * kv)
              + sel(1696 + 64 * i) + sel(2336 + 64 * kv))
    uq = P["mla_w_uq"][l][:, 96 * i:96 * i + 96]
    perm = list(range(80, 96)) + list(range(64, 80))
    wuq = np.concatenate([uq, uq[:, 0:64], uq[:, perm]], 1)
    gq, gk = P["mla_q_g"][l], P["mla_k_g"][l]
    gqp, gkp = np.zeros(96, np.float32), np.zeros(96, np.float32)
    gqp[64:96], gkp[64:96] = gq[perm], gk[perm]
    gcol = np.stack([_col(P["moba_q_g"][l]), _col(P["moba_k_g"][l]), _col(gq), _col(gqp), _col(gk), _col(gkp),
                     np.concatenate([P["dil_q_g"][l], P["swa_q_g"][l]]),
                     np.concatenate([P["dil_k_g"][l], P["swa_k_g"][l]])], 1).astype(np.float32)
    d = dict(ang=np.ascontiguousarray(P["attn_norm_g"][l].reshape(8, 128).T),
             wAB=np.ascontiguousarray(w_in[:, colsAB]), wCD=np.ascontiguousarray(w_in[:, colsCD]),
             wuq=np.ascontiguousarray(wuq), gql=np.ascontiguousarray(P["mla_qlat_g"][l].reshape(2, 128).T),
             wukv=np.ascontiguousarray(P["mla_w_ukv"][l][:, 128 * i:128 * i + 128]),
             gkvl=np.ascontiguousarray(P["mla_kvlat_g"][l].reshape(128, 1)), gcol=np.ascontiguousarray(gcol),
             rope=rope, OH=OH, sink=np.full((128, 1), P["swa_sinks"][l][i], np.float32))
    d.update(_consts(i))
    return d


def kernel(**inputs):
    P = {k: np.asarray(v) for k, v in inputs.items()}
    x = np.ascontiguousarray(P["x"], dtype=np.float32)
    cores = list(range(NCORES))
    ident = np.eye(128, dtype=np.float32).astype(NPBF)
    rope, OH = _shared_consts()
    nc = _prog("fused", build_fused)
    ims = []
    for c in cores:
        b, i = c // 4, c % 4
        d = {"x": np.ascontiguousarray(x[b, 2048 * i:2048 * (i + 1)]), "ident": ident}
        for l in range(2):
            a = _attn_inputs(P, l, i, None, rope, OH)
            for n, _, _ in A_LAYER_INPUTS:
                d["%s_%d" % (n, l)] = a[n]
            for n, _, _ in A_SHARED_INPUTS:
                d[n] = a[n]
            d["w_o_%d" % l] = P["w_o"][l]
            d["gog_%d" % l] = np.ascontiguousarray(P["group_out_g"][l].reshape(8, 128).T)
            d["mlg_%d" % l] = np.ascontiguousarray(P["mlp_norm_g"][l].reshape(8, 128).T)
            d["w_up_%d" % l] = P["w_up"][l]
            d["w_down_%d" % l] = P["w_down"][l]
        ims.append({k: v for k, v in d.items() if k in _NEEDED})
    res = run_bass_kernel_spmd(nc, ims, core_ids=cores)
    out = np.zeros((2, S, D), np.float32)
    for c in cores:
        out[c // 4, 2048 * (c % 4):2048 * (c % 4 + 1)] = np.asarray(res.results[c]["xo"])
    return out
```

```python
import contextlib
import numpy as np
import ml_dtypes
import concourse.bass as bass
import concourse.mybir as mybir
from concourse.bass_utils import run_bass_kernel_spmd

F32 = mybir.dt.float32
BF16 = mybir.dt.bfloat16
AF = mybir.ActivationFunctionType
ALU = mybir.AluOpType
AX = mybir.AxisListType
NPBF = ml_dtypes.bfloat16

S = 8192
D = 1024
EPS = 1e-6
BIG = 240000.0
NCORES = 8


class Buf:
    __slots__ = ("name", "w", "r", "multi")

    def __init__(self, name, multi=False):
        self.name = name
        self.w = {}
        self.r = {}
        self.multi = multi


class Op:
    __slots__ = ("eng", "fn", "deps", "marked", "val", "dma", "sem", "semval", "uid")


class Prog:
    ENG = ("pe", "act", "dve", "pool", "sp")
    NDSEM = 8

    def __init__(self, nc, stack):
        self.nc = nc
        self.ops = {e: [] for e in self.ENG}
        self.esem = {e: stack.enter_context(nc.semaphore("es_" + e)) for e in self.ENG}
        self.dsem = {e: [stack.enter_context(nc.semaphore("ds_%s%d" % (e, i))) for i in range(self.NDSEM)]
                     for e in ("sp", "pool", "act")}
        self.dsem["cc"] = [stack.enter_context(nc.semaphore("ds_cc%d" % i)) for i in range(4)]
        self.dtot = {e: [0] * len(v) for e, v in self.dsem.items()}
        self.drr = {e: 0 for e in self.dsem}
        self.uid = 0

    def add(self, eng, fn, r=(), w=(), dma=False, cc=False):
        op = Op()
        op.eng, op.fn, op.dma, op.marked, op.val = eng, fn, dma, False, 0
        self.uid += 1
        op.uid = self.uid
        deps = set()
        for b in r:
            deps.update(b.w.values())
        for b in w:
            deps.update(b.r.values())
            if not b.multi:
                deps.update(b.w.values())
        key = ("d", op.uid) if dma else eng
        for b in r:
            b.r[key] = op
        for b in w:
            if b.multi:
                b.w[key] = op
            else:
                b.w = {key: op}
                b.r = {}
        if eng == "pe":
            deps = {d for d in deps if d.dma or d.eng != "pe"}
        op.deps = deps
        for d in deps:
            if not d.dma:
                d.marked = True
        if dma:
            se = "cc" if cc else eng
            k = self.drr[se]
            self.drr[se] = (k + 1) % len(self.dsem[se])
            op.sem = (se, k)
            self.dtot[se][k] += 1 if cc else 16
            op.semval = self.dtot[se][k]
            op.val = 1 if cc else 16
        self.ops[eng].append(op)
        return op

    def emit(self):
        nc = self.nc
        if not hasattr(self, "cnt"):
            self.cnt = {e: 0 for e in self.ENG}
            self.waited = {e: {} for e in self.ENG}
            self.prev_last = {e: 0 for e in self.ENG}
            self.prev_dtot = {e: [0] * len(v) for e, v in self.dtot.items()}
        for e in self.ENG:
            last = None
            for op in self.ops[e]:
                if not op.dma:
                    last = op
            if last is not None:
                last.marked = True
            for op in self.ops[e]:
                if op.marked and not op.dma:
                    self.cnt[e] += 1
                    op.val = self.cnt[e]
        names = {"pe": "tensor", "act": "scalar", "dve": "vector", "pool": "gpsimd", "sp": "sync"}
        prev_last = dict(self.prev_last)
        prev_dtot = {e: list(v) for e, v in self.prev_dtot.items()}
        with nc.Block() as block:
            for e in self.ENG:
                def body(eng, e=e):
                    waited = self.waited[e]

                    def wait(sem, key, val):
                        if waited.get(key, 0) < val:
                            eng.wait_ge(sem, val)
                            waited[key] = val
                    for e2 in self.ENG:
                        if e2 != e and prev_last[e2]:
                            wait(self.esem[e2], e2, prev_last[e2])
                    for e2, tots in prev_dtot.items():
                        if e2 == "cc":
                            continue
                        for k, tot in enumerate(tots):
                            if tot:
                                wait(self.dsem[e2][k], (e2, k), tot)
                    self.cur_eng = e
                    for op in self.ops[e]:
                        for d in op.deps:
                            if d.dma:
                                wait(self.dsem[d.sem[0]][d.sem[1]], d.sem, d.semval)
                            else:
                                wait(self.esem[d.eng], d.eng, d.val)
                        if op.dma:
                            wait(self.dsem[op.sem[0]][op.sem[1]], op.sem, op.semval - op.val)
                        ins = op.fn(eng)
                        if op.dma:
                            if op.val == 1:
                                ins.then_inc(self.dsem[op.sem[0]][op.sem[1]])
                            else:
                                ins.then_inc(self.dsem[op.sem[0]][op.sem[1]], 16)
                        elif op.marked:
                            ins.then_inc(self.esem[e], 1)
                    for e2 in self.dsem:
                        own = (e2 == e)
                        if own:
                            for k in range(len(self.dsem[e2])):
                                if self.dtot[e2][k]:
                                    wait(self.dsem[e2][k], (e2, k), self.dtot[e2][k])
                getattr(block, names[e])(body)
        for e in self.ENG:
            self.prev_last[e] = self.cnt[e]
            self.ops[e] = []
        self.prev_dtot = {e: list(v) for e, v in self.dtot.items()}


class Ring:
    def __init__(self, items):
        self.items = items
        self.i = 0

    def next(self):
        it = self.items[self.i]
        self.i = (self.i + 1) % len(self.items)
        return it


class Ctx:
    def __init__(self, nc, stack):
        self.nc, self.stack = nc, stack
        self.P = Prog(nc, stack)
        self.n = 0
        self._pid = {}

    def pid(self, e, fn, tag):
        ph = self.P.cnt["pe"] if hasattr(self.P, "cnt") else -1
        k0 = (id(e), ph)
        if k0 not in self._pid:
            self._pid[k0] = e.partition_id()
        k = (id(e), ph, tag)
        if k not in self._pid:
            self._pid[k] = fn(self._pid[k0])
        return self._pid[k]

    def sb(self, shape, dt, name=None, multi=False):
        self.n += 1
        nm = "%s_%d" % (name or "t", self.n)
        t = self.stack.enter_context(self.nc.sbuf_tensor(nm, list(shape), dt))
        return t, Buf(nm, multi)

    def sbring(self, k, shape, dt, name=None):
        return Ring([self.sb(shape, dt, name) for _ in range(k)])

    def ps(self, dt=F32, name=None):
        self.n += 1
        nm = "%s_%d" % (name or "ps", self.n)
        shape = [128, 512] if dt == F32 else [128, 1024]
        t = self.stack.enter_context(self.nc.psum_tensor(nm, shape, dt))
        return t, Buf(nm)

    def dram(self, name, shape, dt, kind):
        return self.nc.dram_tensor(name, list(shape), dt, kind=kind).ap()

    def dma(self, eng, out, in_, r=(), w=()):
        return self.P.add(eng, lambda e: e.dma_start(out=out, in_=in_), r, w, dma=True)

    def mm(self, out, lhsT, rhs, start, stop, r, w):
        return self.P.add("pe", lambda e: e.matmul(out, lhsT, rhs, start=start, stop=stop), r, w)

    def tr(self, out, in_, ident, r, w):
        return self.P.add("pe", lambda e: e.transpose(out, in_, ident), r, w)

    def act(self, out, in_, func, r, w, bias=0.0, scale=1.0, eng="act"):
        return self.P.add(eng, lambda e: e.activation(out, in_, func, bias=bias, scale=scale), r, w)

    def copy(self, eng, out, in_, r, w):
        if eng == "act":
            return self.P.add("act", lambda e: e.copy(out, in_), r, w)
        return self.P.add(eng, lambda e: e.tensor_copy(out, in_), r, w)

    def tt(self, eng, out, in0, in1, op, r, w):
        return self.P.add(eng, lambda e: e.tensor_tensor(out, in0, in1, op), r, w)

    def ts(self, eng, out, in0, s1, op0, r, w, s2=None, op1=None):
        if op1 is None:
            return self.P.add(eng, lambda e: e.tensor_scalar(out, in0, s1, None, op0), r, w)
        return self.P.add(eng, lambda e: e.tensor_scalar(out, in0, s1, s2, op0, op1), r, w)

    def stt(self, eng, out, in0, scalar, in1, op0, op1, r, w):
        return self.P.add(eng, lambda e: e.scalar_tensor_tensor(out, in0, scalar, in1, op0, op1), r, w)

    def recip(self, out, in_, r, w):
        return self.P.add("dve", lambda e: e.reciprocal(out, in_), r, w)

    def memset(self, eng, ap, val, w):
        return self.P.add(eng, lambda e: e.memset(ap, val), (), w)


def phase_M(C, G, l, do_layer, do_norm, x_d, x_db, xo_d, xo_db):
    out_x = xo_d is not None
    with contextlib.ExitStack() as st:
        C.stack = st
        id_d = G["ident"]
        if do_layer:
            ot_all = G["OT_all"]
            wo_d, gog_d, mlg_d, wup_d, wdn_d = (G["%s_%d" % (n, l)] for n in ("w_o", "gog", "mlg", "w_up", "w_down"))
        xnT_d = G["xnT_own"]

        X, _ = C.sb([128, 16, D], F32, "X")
        Xb = [Buf("X%d" % i) for i in range(16)]
        ident, identb = C.sb([128, 128], BF16, "ident")
        C.dma("sp", ident[:, :], id_d[:, :], (), (identb,))
        xv = x_d.rearrange("(b p) d -> p b d", p=128)
        for tb in range(16):
            C.dma("sp" if tb % 2 == 0 else "pool", X[:, tb, :], xv[:, tb, :], (x_db,), (Xb[tb],))
        AT, _ = C.sb([128, 8, 2048], BF16, "AT")
        ATb = [Buf("AT%d" % i) for i in range(16)]
        pst = [C.ps(BF16, "pst") for _ in range(2)]
        pstr = Ring(pst)
        psf = Ring([C.ps(F32, "psf") for _ in range(6)])
        sq_r = C.sbring(2, [128, D], F32, "sq")
        xnb_r = C.sbring(2, [128, D], BF16, "xnb")
        st_r = C.sbring(4, [128, 4], F32, "stat")

        def norm_T(tb, dstT, dstb):
            sq, sqb = sq_r.next()
            C.act(sq[:, :], X[:, tb, :], AF.Square, (Xb[tb],), (sqb,))
            s, sb_ = st_r.next()
            C.P.add("dve", lambda e: e.reduce_sum(s[:, 0:1], sq[:, :], AX.X), (sqb,), (sb_,))
            C.act(s[:, 1:2], s[:, 0:1], AF.Sqrt, (sb_,), (sb_,), bias=EPS, scale=1.0 / D)
            C.recip(s[:, 2:3], s[:, 1:2], (sb_,), (sb_,))
            xnb, xnbb = xnb_r.next()
            C.ts("dve", xnb[:, :], X[:, tb, :], s[:, 2:3], ALU.mult, (Xb[tb], sb_), (xnbb,))
            pt, ptb = pstr.next()
            for kc in range(8):
                C.tr(pt[:, kc * 128:(kc + 1) * 128], xnb[:, kc * 128:(kc + 1) * 128], ident[:, :],
                     (xnbb, identb), (ptb,))
            C.copy("act", dstT[:, :, tb * 128:(tb + 1) * 128],
                   pt[:, :].rearrange("p (k t) -> p k t", k=8), (ptb,), dstb if isinstance(dstb, tuple) else (dstb,))

        if do_layer:
            gog, gogb = C.sb([128, 8], F32, "gog")
            mlg, mlgb = C.sb([128, 8], F32, "mlg")
            C.dma("sp", gog[:, :], gog_d[:, :], (), (gogb,))
            C.dma("sp", mlg[:, :], mlg_d[:, :], (), (mlgb,))
            ones, onesb = C.sb([128, 1], BF16, "ones")
            C.memset("pool", ones[:, :], 1.0, (onesb,))
            if True:
                WU, wob = C.sb([128, 8192], BF16, "WU", multi=True)
                wo = WU[:, :].rearrange("p (k n) -> p k n", k=8)
                stg_r = C.sbring(2, [128, D], F32, "wostg")
                wov = wo_d.rearrange("(k p) n -> p k n", p=128)
                for kc in range(8):
                    sg, sgb = stg_r.next()
                    C.dma("sp", sg[:, :], wov[:, kc, :], (), (sgb,))
                    C.P.add("act", lambda e, kc=kc, sg=sg: e.activation(wo[:, kc, :], sg[:, :], AF.Copy, scale=gog[:, kc:kc + 1]),
                            (sgb, gogb), (wob,))
                ATm = [Buf("ATm%d" % m) for m in range(4)]
                for m in (1, 0, 3, 2):
                    otv = ot_all[m].rearrange("(a r) t -> r a t", a=2)
                    for b2 in range(2):
                        def f(e, m=m, b2=b2, otv=otv):
                            off = C.pid(e, lambda p: (p % 4) * 2048, "i4")
                            return e.dma_start(out=AT[b2 * 64:(b2 + 1) * 64, 2 * m:2 * m + 2, :],
                                               in_=otv[b2 * 64:(b2 + 1) * 64, :, bass.ds(off, 2048)])
                        C.P.add("sp", f, (G["OT_all_b"][m],), (ATm[m],), dma=True)
                sqo_r = C.sbring(2, [128, 8, 128], BF16, "sqo")
                for gs in ((0, 1, 3), (2,)):
                    for tb in range(16):
                        tsl = slice(tb * 128, (tb + 1) * 128)
                        sqo, sqob = sqo_r.next()
                        for g in gs:
                            C.tt("pool", sqo[:, 2 * g:2 * g + 2, :], AT[:, 2 * g:2 * g + 2, tsl],
                                 AT[:, 2 * g:2 * g + 2, tsl], ALU.mult, (ATm[g],), (sqob,))
                        pss, pssb = psf.next()
                        for g in gs:
                            for j in range(2):
                                C.mm(pss[:, g:g + 1], sqo[:, 2 * g + j, :], ones[:, :], j == 0, j == 1,
                                     (sqob, onesb), (pssb,))
                        s, sb_ = st_r.next()
                        rs, rsb = st_r.next()
                        cl = slice(0, 4) if len(gs) > 1 else slice(2, 3)
                        C.act(s[:, cl], pss[:, cl], AF.Sqrt, (pssb,), (sb_,), bias=EPS, scale=1.0 / 256)
                        C.recip(rs[:, cl], s[:, cl], (sb_,), (rsb,))
                        for half in range(2):
                            hs = slice(half * 512, (half + 1) * 512)
                            for g in gs:
                                pg, pgb = psf.next()
                                for j in range(2):
                                    C.mm(pg[:, :], AT[:, 2 * g + j, tsl], wo[:, 2 * g + j, hs], j == 0, j == 1,
                                         (ATm[g], wob), (pgb,))
                                C.stt("dve", X[:, tb, hs], pg[:, :], rs[:, g:g + 1], X[:, tb, hs], ALU.mult, ALU.add,
                                      (pgb, rsb, Xb[tb]), (Xb[tb],))
            if True:
                for tb in range(16):
                    norm_T(tb, AT, (ATb[tb],) + tuple(ATm))
                ustg_r = C.sbring(3, [128, 2, 512], F32, "ustg")
                dstg_r = C.sbring(2, [128, 1024], F32, "dstg")
                wup_r = Ring([C.sb([128, 8, 512], BF16, "wup", multi=True) for _ in range(2)])
                wdn_r = Ring([C.sb([128, 4, D], BF16, "wdn", multi=True) for _ in range(2)])
                uT = WU[:, :].rearrange("p (k n) -> p k n", k=4)
                uTb = [[Buf("uT%d_%d" % (a, b)) for b in range(4)] for a in range(4)]
                sq2_r = C.sbring(2, [128, 512], F32, "sq2")
                wupv = wup_d.rearrange("(k p) n -> p k n", p=128)
                wdnv = wdn_d.rearrange("(k p) n -> p k n", p=128)
                for fg in range(8):
                    wup, wupb = wup_r.next()
                    wdn, wdnb = wdn_r.next()
                    for k2 in range(4):
                        sg, sgb = ustg_r.next()
                        C.dma("sp", sg[:, :, :], wupv[:, 2 * k2:2 * k2 + 2, fg * 512:(fg + 1) * 512], (), (sgb,))
                        for j in range(2):
                            kc = 2 * k2 + j
                            C.P.add("act", lambda e, kc=kc, sg=sg, j=j, wup=wup: e.activation(
                                wup[:, kc, :], sg[:, j, :], AF.Copy, scale=mlg[:, kc:kc + 1]), (sgb, mlgb), (wupb,))
                    for fc in range(4):
                        sg, sgb = dstg_r.next()
                        C.dma("pool", sg[:, :], wdnv[:, fg * 4 + fc, :], (), (sgb,))
                        C.copy("act" if fc % 2 else "dve", wdn[:, fc, :], sg[:, :], (sgb,), (wdnb,))
                    for fc in range(4):
                        for tg in range(4):
                            pu, pub = psf.next()
                            for kc in range(8):
                                C.mm(pu[:, :], wup[:, kc, fc * 128:(fc + 1) * 128],
                                     AT[:, kc, tg * 512:(tg + 1) * 512], kc == 0, kc == 7,
                                     (wupb,) + tuple(ATb[4 * tg:4 * tg + 4]), (pub,))
                            sq2, sq2b = sq2_r.next()
                            C.act(sq2[:, :], pu[:, :], AF.Square, (pub,), (sq2b,))
                            C.stt("dve", uT[:, fc, tg * 512:(tg + 1) * 512], pu[:, :], 0.0, sq2[:, :],
                                  ALU.is_gt, ALU.mult, (pub, sq2b), (uTb[fc][tg], wob))
                    for tb in range(16):
                        tsl = slice(tb * 128, (tb + 1) * 128)
                        for half in range(2):
                            hs = slice(half * 512, (half + 1) * 512)
                            pd, pdb = psf.next()
                            for fc in range(4):
                                C.mm(pd[:, :], uT[:, fc, tsl], wdn[:, fc, hs], fc == 0, fc == 3,
                                     (uTb[fc][tb // 4], wdnb), (pdb,))
                            C.tt("dve", X[:, tb, hs], pd[:, :], X[:, tb, hs], ALU.add, (pdb, Xb[tb]), (Xb[tb],))
        if out_x:
            xov = xo_d.rearrange("(b p) d -> p b d", p=128)
            for tb in range(16):
                C.dma("sp" if tb % 2 == 0 else "pool", xov[:, tb, :], X[:, tb, :], (Xb[tb],), (xo_db,))
        if do_norm:
            for q in range(4):
                for tb in range(4 * q, 4 * q + 4):
                    norm_T(tb, AT, ATb[tb])
                xnv = xnT_d[q].rearrange("(k p) t -> p k t", p=128)
                C.dma("sp", xnv, AT[:, :, q * 512:(q + 1) * 512],
                      tuple(ATb[4 * q:4 * q + 4]), (G["xnT_own_b"][q],))
                allgather(C, G["xnT_own_t"][q], G["xnT_own_b"][q], G["xnT_all_t"][q], G["xnT_all_b"][q])
        C.P.emit()


def allgather(C, src_t, src_b, dst_t, dst_b):
    C.P.add("pool", lambda e: e.collective_compute(
        "AllGather", ALU.bypass, replica_groups=[[0, 1, 2, 3], [4, 5, 6, 7]],
        ins=[src_t.ap().opt()], outs=[dst_t.ap().opt()]), (src_b,), (dst_b,), dma=True, cc=True)


NAB = 640
NCD = 384


A_LAYER_INPUTS = (("ang", [128, 8], F32), ("wAB", [D, NAB], F32), ("wCD", [D, NCD], F32), ("wuq", [256, 192], F32),
                  ("gql", [128, 2], F32), ("wukv", [128, 128], F32), ("gkvl", [128, 1], F32), ("gcol", [128, 8], F32),
                  ("sink", [128, 1], F32))
A_SHARED_INPUTS = (("rope", [64, S], F32), ("T2", [128, 128], F32), ("PM", [128, 128], F32), ("kbias", [128, 2], F32),
                   ("OH", [32, S], BF16), ("cst", [128, 512], BF16), ("Wt", [128, 1024], BF16), ("SEL", [128, 64], F32))


def phase_A(C, G, l):
    with contextlib.ExitStack() as st:
        C.stack = st
        (ang_d, wAB_d, wCD_d, wuq_d, gql_d, wukv_d, gkvl_d, gcol_d, esk_d) = (G["%s_%d" % (n, l)] for n, _, _ in A_LAYER_INPUTS)
        (rope_d, T2_d, PM_d, kb_d, OH_d, cst_d, W_d, sel_d) = (G[n] for n, _, _ in A_SHARED_INPUTS)
        xnT_all, xnT_all_b = G["xnT_all"], G["xnT_all_b"]
        OT_d, OT_b = G["OT_own"], G["OT_own_b"]

        ld = lambda shape, dt, src, nm, eng="sp": _load(C, shape, dt, src, nm, eng)
        ang, angb = ld([128, 8], F32, ang_d[:, :], "ang")
        gql, gqlb = ld([128, 2], F32, gql_d[:, :], "gql")
        gkvl, gkvlb = ld([128, 1], F32, gkvl_d[:, :], "gkvl")
        gcol, gcolb = ld([128, 8], F32, gcol_d[:, :], "gcol")
        T2, T2b = ld([128, 128], F32, T2_d[:, :], "T2")
        PM, PMb = ld([128, 128], F32, PM_d[:, :], "PM")
        kbias, kbb = ld([128, 2], F32, kb_d[:, :], "kbias")
        cst, cstb = ld([128, 512], BF16, cst_d[:, :], "cst")
        Wt, Wtb = ld([128, 1024], BF16, W_d[:, :], "Wt")
        SEL, SELb = ld([128, 64], F32, sel_d[:, :], "SEL")
        esk, eskb = ld([128, 1], F32, esk_d[:, :], "esk")
        C.act(esk[:, :], esk[:, :], AF.Exp, (eskb,), (eskb,))
        epsc, epsb = C.sb([128, 1], F32, "epsc")
        C.memset("pool", epsc[:, :], EPS, (epsb,))
        ident = cst[:, 0:128]
        TRI = cst[:, 128:256]
        BD = cst[:, 256:384]
        ONES = cst[:, 384:512]

        T1, T1b = C.sb([128, S], BF16, "T1", multi=True)
        T2t, T2tb = C.sb([128, S], BF16, "T2t", multi=True)
        T3, T3b = C.sb([128, S], BF16, "T3", multi=True)
        T4, T4b = C.sb([128, S], BF16, "T4", multi=True)
        Vt = [C.sb([128, 64, 65], BF16, "V%d" % i, multi=True) for i in range(4)]
        for (v, vb) in Vt:
            C.memset("pool", v[:, :, :], 1.0, (vb,))
        RAWOCT, _ = C.sb([128, 6 * 512], F32, "RAWOCT")
        OCT, OCTb = RAWOCT[:, 0:2048], Buf("OCT")
        raw_items = [(RAWOCT[:, i * 512:(i + 1) * 512], Buf("raw%d" % i)) for i in range(6)]

        wAB, wABb = C.sb([128, 8, NAB], BF16, "wAB", multi=True)
        wCD, wCDb = C.sb([128, 8, NCD], BF16, "wCD", multi=True)
        wuq, wuqb = C.sb([128, 2, 192], BF16, "wuq", multi=True)
        wukv, wukvb = C.sb([128, 128], BF16, "wukv")
        wABv = wAB_d.rearrange("(k p) n -> p k n", p=128)
        wCDv = wCD_d.rearrange("(k p) n -> p k n", p=128)
        wuqv = wuq_d.rearrange("(k p) n -> p k n", p=128)
        wload = []

        def stage_w(dst, dstb, src, ncols, scol, scolb, nk, q):
            for kc in range(nk):
                for c0 in range(0, ncols, 512):
                    n = min(512, ncols - c0)
                    sg, sgb = stg_ring.next()
                    sv = src[:, kc, c0:c0 + n] if nk > 1 or len(src.shape) == 3 else src[:, c0:c0 + n]
                    dv = dst[:, kc, c0:c0 + n] if len(dst.shape) == 3 else dst[:, c0:c0 + n]
                    C.dma(q, sg[:, 0:n], sv, (), (sgb,))
                    C.ts("dve", dv, sg[:, 0:n], scol[:, kc:kc + 1], ALU.mult, (sgb, scolb), (dstb,))

        psA = Ring([C.ps(F32, "psA") for _ in range(3)])
        psB = Ring([C.ps(F32, "psB") for _ in range(2)])
        psC = Ring([C.ps(F32, "psC") for _ in range(2)])
        psT, psTb = C.ps(BF16, "psT")
        xn_r = C.sbring(2, [128, 8, 512], BF16, "xn")
        f_r = C.sbring(6, [128, 512], F32, "f")
        qr_r = C.sbring(4, [128, 512], F32, "qr")
        vt_r = C.sbring(4, [128, 512], BF16, "vt")
        h_r = C.sbring(4, [128, 512], BF16, "h")
        rp_r = C.sbring(2, [128, 2, 512], F32, "rp")
        xnv4 = [a.rearrange("(r k p) t -> r p k t", r=4, p=128) for a in xnT_all]

        def load_xn(c):
            xn, xnb = xn_r.next()
            src = xnv4[c % 4][c // 4]
            C.dma("sp", xn[:, 0:4, :], src[:, 0:4, :], (xnT_all_b[c % 4],), (xnb,))
            C.dma("pool", xn[:, 4:8, :], src[:, 4:8, :], (xnT_all_b[c % 4],), (xnb,))
            return xn, xnb
        stg_ring = Ring(f_r.items + raw_items)
        stage_w(wAB, wABb, wABv, NAB, ang, angb, 8, "act")
        stage_w(wuq, wuqb, wuqv, 192, gql, gqlb, 2, "act")
        stage_w(wukv, wukvb, wukv_d, 128, gkvl, gkvlb, 1, "act")
        ksum, ksumb = C.sb([64, 32], F32, "ksum")
        C_qlb = C.sbring(4, [128, 2, 512], BF16, "ql")

        def proj(xn, xnb, c0, ncol):
            p, pb = psA.next()
            for kc in range(8):
                C.mm(p[0:ncol, :], wsrc[0][:, kc, c0:c0 + ncol], xn[:, kc, :], kc == 0, kc == 7,
                     (wsrc[1], xnb), (pb,))
            return p, pb

        def rstd_of(src, srcb, lo, hi, ones_ap, n, extra=None):
            h, hb = h_r.next()
            C.act(h[lo:hi, :], src[lo:hi, :], AF.Square, (srcb,), (hb,))
            p, pb = psB.next()
            C.mm(p[lo:hi, :], ones_ap, h[lo:hi, :], True, True, (hb, cstb), (pb,))
            f, fb = f_r.next()
            C.act(f[lo:hi, :], p[lo:hi, :], AF.Ln, (pb, epsb), (fb,), bias=epsc[lo:hi, 0:1], scale=1.0 / n)
            C.act(f[lo:hi, :], f[lo:hi, :], AF.Exp, (fb,), (fb,), scale=-0.5)
            return f, fb

        def vtrans(vT, vTb, base, tok0, stride, V, Vb, blk0, nblk):
            for j in range(nblk):
                a = tok0 + j * 128 * stride
                C.tr(psT[:, j * 64:(j + 1) * 64], vT[base:base + 64, a:a + 127 * stride + 1:stride],
                     cst[base:base + 64, base:base + 64], (vTb, cstb), (psTb,))
            C.copy("act", V[:, blk0:blk0 + nblk, 0:64],
                   psT[:, 0:nblk * 64].rearrange("p (j d) -> p j d", j=nblk), (psTb,), (Vb,))

        QA, KA, QB, KB = T1, T2t, T3, T4
        VA, VAb = Vt[0]
        VB, VBb = Vt[1]
        C.dma("sp", KA[64:96, :], OH_d[:, :], (), (T2tb,))
        wsrc = [wAB, wABb]

        raw_r = Ring(raw_items)
        hk_r = C.sbring(4, [128, 512], BF16, "hk")

        def stat(h_ap, hb, ones_ap, lo, hi, n, k2=None):
            p, pb = psB.next()
            if k2 is None:
                C.mm(p[lo:hi, :], ones_ap, h_ap, True, True, (hb, cstb), (pb,))
            else:
                for j in range(2):
                    C.mm(p[lo:hi, :], ones_ap, k2[:, j, :], j == 0, j == 1, (hb, cstb), (pb,))
            f, fb = f_r.next()
            C.act(f[lo:hi, :], p[lo:hi, :], AF.Ln, (pb, epsb), (fb,), bias=epsc[lo:hi, 0:1], scale=1.0 / n)
            C.act(f[lo:hi, :], f[lo:hi, :], AF.Exp, (fb,), (fb,), scale=-0.5)
            return f, fb

        def stageA(c, mid=None):
            cs = slice(c * 512, (c + 1) * 512)
            xn, xnb = load_xn(c)
            rp, rpb = rp_r.next()
            C.dma("sp", rp[64:96, 0, :], rope_d[0:32, cs], (), (rpb,))
            C.dma("sp", rp[64:96, 1, :], rope_d[32:64, cs], (), (rpb,))
            raws = []
            for (c0, n) in ((0, 96), (96, 64), (160, 96)):
                p, pb = proj(xn, xnb, c0, n)
                rw, rwb = raw_r.next()
                C.copy("act", rw[0:n, :], p[0:n, :], (pb,), (rwb,))
                raws.append((rw, rwb))
            if mid is not None:
                mid()
            qlb, qlbb = C_qlb.next()
            hq, hqb = C_qlb.next()
            for j in range(2):
                p, pb = proj(xn, xnb, 256 + 128 * j, 128)
                C.copy("act", qlb[:, j, :], p[:, :], (pb,), (qlbb,))
                C.tt("dve", hq[:, j, :], p[:, :], qlb[:, j, :], ALU.mult, (pb, qlbb), (hqb,))
            p5, p5b = proj(xn, xnb, 512, 128)
            kvb, kvbb = hk_r.next()
            hkv, hkvb = hk_r.next()
            C.copy("act", kvb[:, :], p5[:, :], (p5b,), (kvbb,))
            C.tt("dve", hkv[:, :], p5[:, :], kvb[:, :], ALU.mult, (p5b, kvbb), (hkvb,))
            (r3, r3b) = raws[2]
            rl, rlb = stat(None, hqb, ONES, 0, 128, 256, k2=hq)
            rkv, rkvb = stat(hkv[:, :], hkvb, ONES, 0, 128, 128)
            P1, P1b = psC.next()
            for j in range(2):
                C.mm(P1[0:96, :], wuq[:, j, 0:96], qlb[:, j, :], j == 0, j == 1, (wuqb, qlbb), (P1b,))
            QR, QRb = qr_r.next()
            C.tt("dve", QR[0:96, :], P1[0:96, :], rl[0:96, :], ALU.mult, (P1b, rlb), (QRb,))
            P2, P2b = psC.next()
            for j in range(2):
                C.mm(P2[0:96, :], wuq[:, j, 96:192], qlb[:, j, :], j == 0, j == 1, (wuqb, qlbb), (P2b,))
            QRP, QRPb = qr_r.next()
            C.tt("dve", QRP[64:96, :], P2[64:96, :], rl[64:96, :], ALU.mult, (P2b, rlb), (QRPb,))
            vT, vTb = vt_r.next()
            C.copy("pool", vT[0:64, :], r3[0:64, :], (r3b,), (vTb,))
            Pk, Pkb = psC.next()
            C.mm(Pk[0:64, :], wukv[:, 0:64], kvb[:, :], True, True, (wukvb, kvbb), (Pkb,))
            C.tt("dve", r3[0:64, :], Pk[0:64, :], rkv[0:64, :], ALU.mult, (Pkb, rkvb, vTb), (r3b,))
            Pv, Pvb = psC.next()
            C.mm(Pv[0:64, :], wukv[:, 64:128], kvb[:, :], True, True, (wukvb, kvbb), (Pvb,))
            vT2, vT2b = vt_r.next()
            C.tt("dve", vT2[0:64, :], Pv[0:64, :], rkv[0:64, :], ALU.mult, (Pvb, rkvb), (vT2b,))
            return dict(c=c, rp=(rp, rpb), raws=raws, QR=(QR, QRb), QRP=(QRP, QRPb), vT=(vT, vTb), vT2=(vT2, vT2b))

        def stageB(S_):
            c = S_["c"]
            cs = slice(c * 512, (c + 1) * 512)
            rp, rpb = S_["rp"]
            (r1, r1b), (r2, r2b), (r3, r3b) = S_["raws"]
            QR, QRb = S_["QR"]
            QRP, QRPb = S_["QRP"]
            vT, vTb = S_["vT"]
            vT2, vT2b = S_["vT2"]
            vtrans(vT, vTb, 0, 0, 1, VA, VAb, 4 * c, 4)
            vtrans(vT2, vT2b, 0, 0, 1, VB, VBb, 4 * c, 4)
            hs = []
            for (src, srcb, lo, hi) in ((r1, r1b, 0, 64), (r2, r2b, 0, 64), (QR, QRb, 0, 96), (r3, r3b, 0, 96)):
                h, hb = h_r.next()
                C.act(h[lo:hi, :], src[lo:hi, :], AF.Square, (srcb,), (hb,))
                hs.append((h, hb))
            rsq, rsqb = stat(hs[0][0][0:64, :], hs[0][1], ONES[0:64, 0:64], 0, 64, 64)
            rsk, rskb = stat(hs[1][0][0:64, :], hs[1][1], ONES[0:64, 0:64], 0, 64, 64)
            rq, rqb = stat(hs[2][0][0:96, :], hs[2][1], ONES[0:96, 0:96], 0, 96, 96)
            rk, rkb = stat(hs[3][0][0:96, :], hs[3][1], ONES[0:96, 0:96], 0, 96, 96)
            C.stt("dve", QA[0:64, cs], r1[0:64, :], gcol[0:64, 0:1], rsq[0:64, :], ALU.mult, ALU.mult,
                  (r1b, gcolb, rsqb), (T1b,))
            C.stt("dve", r2[0:64, :], r2[0:64, :], gcol[0:64, 1:2], rsk[0:64, :], ALU.mult, ALU.mult,
                  (r2b, gcolb, rskb), (r2b,))
            C.copy("pool", KA[0:64, cs], r2[0:64, :], (r2b,), (T2tb,))
            C.P.add("dve", lambda e, r2=r2, c=c: e.reduce_sum(
                ksum[0:64, 2 * c:2 * c + 2], r2[0:64, :].rearrange("p (a b) -> p a b", a=2), AX.X),
                (r2b,), (ksumb,))
            C.stt("dve", QB[0:64, cs], QR[0:64, :], gcol[0:64, 2:3], rq[0:64, :], ALU.mult, ALU.mult,
                  (QRb, gcolb, rqb), (T3b,))
            C.stt("dve", QR[64:96, :], QR[64:96, :], gcol[64:96, 2:3], rp[64:96, 0, :], ALU.mult, ALU.mult,
                  (QRb, gcolb, rpb), (QRb,))
            C.stt("dve", QRP[64:96, :], QRP[64:96, :], gcol[64:96, 3:4], rp[64:96, 1, :], ALU.mult, ALU.mult,
                  (QRPb, gcolb, rpb), (QRPb,))
            C.tt("pool", QR[64:96, :], QR[64:96, :], QRP[64:96, :], ALU.add, (QRb, QRPb), (QRb,))
            C.tt("pool", QB[64:96, cs], QR[64:96, :], rq[64:96, :], ALU.mult, (QRb, rqb), (T3b,))
            C.stt("dve", KB[0:64, cs], r3[0:64, :], gcol[0:64, 4:5], rk[0:64, :], ALU.mult, ALU.mult,
                  (r3b, gcolb, rkb), (T4b,))
            C.stt("dve", r3[64:96, :], r3[64:96, :], gcol[64:96, 4:5], rp[64:96, 0, :], ALU.mult, ALU.mult,
                  (r3b, gcolb, rpb), (r3b,))
            C.stt("dve", r1[64:96, :], r1[64:96, :], gcol[64:96, 5:6], rp[64:96, 1, :], ALU.mult, ALU.mult,
                  (r1b, gcolb, rpb), (r1b,))
            C.tt("pool", r3[64:96, :], r3[64:96, :], r1[64:96, :], ALU.add, (r3b, r1b), (r3b,))
            C.tt("pool", KB[64:96, cs], r3[64:96, :], rk[64:96, :], ALU.mult, (r3b, rkb), (T4b,))

        prevS = None
        for c in [r * 4 + j for j in range(4) for r in range(4)]:
            curS = stageA(c, (lambda p=prevS: stageB(p)) if prevS is not None else None)
            prevS = curS
        stageB(prevS)

        stg_ring = Ring(f_r.items)
        stage_w(wCD, wCDb, wCDv, NCD, ang, angb, 8, "sp")

        E_r = C.sbring(4, [128, 512], BF16, "E")
        Osb_r = C.sbring(2, [128, 512], F32, "Osb")
        oo_r = C.sbring(2, [64, 512], BF16, "oo")

        def finalize(Osrc, Osrcb, m, col0, sink=False):
            pd, pdb = psC.next()
            C.mm(pd[0:64, :], SEL[0:65, 0:64], Osrc, True, True, (Osrcb, SELb), (pdb,))
            rc, rcb = f_r.next()
            if sink:
                C.act(rc[0:64, :], pd[0:64, :], AF.Ln, (pdb, eskb), (rcb,), bias=esk[0:64, 0:1], scale=1.0)
            else:
                C.act(rc[0:64, :], pd[0:64, :], AF.Ln, (pdb,), (rcb,))
            C.act(rc[0:64, :], rc[0:64, :], AF.Exp, (rcb,), (rcb,), scale=-1.0)
            oo, oob = oo_r.next()
            C.tt("dve", oo[0:64, :], Osrc[0:64, :], rc[0:64, :], ALU.mult, (Osrcb, rcb), (oob,))
            C.dma("sp", OT_d[m][:, col0:col0 + 512], oo[0:64, :], (oob,), (OT_b[m],))

        class Task:
            __slots__ = ("qk", "ex", "pv", "newO", "fin", "after")

        def run_tasks(tasks, look=2):
            n = len(tasks)
            Sx = [None] * n

            def emit_qk(i):
                Sx[i] = psA.next()
                tasks[i].qk(*Sx[i])
            for i in range(min(look, n)):
                emit_qk(i)
            pend = []
            O = None
            for i, t in enumerate(tasks):
                if i + look < n:
                    emit_qk(i + look)
                E = E_r.next()
                t.ex(Sx[i][0], Sx[i][1], E[0], E[1])
                if t.newO:
                    O = psB.next()
                t.pv(E[0], E[1], O[0], O[1])
                for f in pend:
                    f()
                pend = []
                if t.fin is not None:
                    pend.append(lambda t=t, O=O: t.fin(O[0], O[1]))
                for f in (t.after or ()):
                    f()
            for f in pend:
                f()

        def dense_tasks(Kt, Ktb, Qt, Qtb, rows, V, Vb, scale, bias_of, m):
            tasks = []
            for qt in range(16):
                nkt = 4 * qt + 4
                for kt in range(nkt):
                    j = kt - 4 * qt
                    c0 = 128 * j if j > 0 else 0
                    t = Task()

                    def qk(Sx, Sb, kt=kt, qt=qt, c0=c0):
                        C.mm(Sx[:, c0:512], Kt[0:rows, kt * 128:(kt + 1) * 128],
                             Qt[0:rows, qt * 512 + c0:(qt + 1) * 512], True, True, (Ktb, Qtb), (Sb,))

                    def ex(Sx, Sb, E, Eb, kt=kt, c0=c0, j=j):
                        b = bias_of(kt)
                        rd = (Sb,) if isinstance(b, float) else (Sb, kbb)
                        C.act(E[:, c0:512], Sx[:, c0:512], AF.Exp, rd, (Eb,), bias=b, scale=scale)
                        if j >= 0:
                            C.tt("dve", E[:, c0:c0 + 128], E[:, c0:c0 + 128], TRI, ALU.mult, (Eb, cstb), (Eb,))

                    def pv(E, Eb, O, Ob, kt=kt, c0=c0, nkt=nkt):
                        C.mm(O[0:65, c0:512], V[:, kt, :], E[:, c0:512], kt == 0, kt == nkt - 1, (Vb, Eb), (Ob,))

                    def fin(O, Ob, qt=qt):
                        Osb, Osbb = Osb_r.next()
                        C.copy("act", Osb[0:65, :], O[0:65, :], (Ob,), (Osbb,))
                        finalize(Osb[0:65, :], Osbb, m, qt * 512)
                    t.qk, t.ex, t.pv = qk, ex, pv
                    t.newO = kt == 0
                    t.fin = fin if kt == nkt - 1 else None
                    t.after = None
                    tasks.append(t)
            return tasks

        km, kmb = C.sb([64, 32], BF16, "km")
        C.ts("dve", km[:, :], ksum[:, :], 1.0 / 256, ALU.mult, (ksumb,), (kmb,))
        STg_r = C.sbring(3, [128, 128], BF16, "STg")
        for (stg_, stgb_) in STg_r.items:
            C.memset("pool", stg_[:, :], 0.0, (stgb_,))
        g_r = C.sbring(4, [128, 80], F32, "g")

        def gphase1(qb):
            qblk = qb // 2
            qs = slice(qb * 128, (qb + 1) * 128)
            pg, pgb = psC.next()
            C.mm(pg[:, 0:32], QA[0:64, qs], km[0:64, :], True, True, (T1b, kmb), (pgb,))
            g, gb = g_r.next()
            C.tt("dve", g[:, 0:32], pg[:, 0:32], PM[:, 32 - qblk:64 - qblk], ALU.add, (pgb, PMb), (gb,))
            C.P.add("dve", lambda e: e.max(g[:, 64:72], g[:, 0:32]), (gb,), (gb,))
            C.ts("dve", g[:, 72:73], g[:, 66:67], -1e29, ALU.max, (gb,), (gb,))
            C.ts("dve", g[:, 32:64], g[:, 0:32], g[:, 72:73], ALU.is_ge, (gb,), (gb,))
            C.tt("dve", g[:, 32:64], g[:, 32:64], PM[:, 64 + 32 - qblk:64 + 64 - qblk], ALU.add, (gb, PMb), (gb,))
            C.tt("dve", g[:, 0:32], g[:, 32:64], T2[:, 63 - qb:63 - qb + 64:2], ALU.mult, (gb, T2b), (gb,))
            stg, stgb = STg_r.next()
            C.ts("dve", stg[:, 64:96], g[:, 0:32], -BIG, ALU.add, (gb,), (stgb,))

            def part2():
                C.tr(psT[:, 0:128], stg[:, :], ident, (stgb, cstb), (psTb,))
                C.copy("act", QA[64:96, qs], psT[64:96, 0:128], (psTb,), (T1b,))
            return part2

        def ag_m(m):
            allgather(C, G["OT_own_t"][m], OT_b[m], G["OT_all_t"][m], G["OT_all_b"][m])

        sB = 96 ** -0.5
        tB = dense_tasks(KB, T4b, QB, T3b, 96, VB, VBb, sB, lambda kt: 0.0, 1)
        step = len(tB) // 64
        for qb in range(64):
            i1 = qb * step
            i2 = min(i1 + 5, len(tB) - 1)
            holder = {}

            def f1(qb=qb, holder=holder):
                holder["p2"] = gphase1(qb)

            def f2(holder=holder):
                holder["p2"]()
            tB[i1].after = (tB[i1].after or ()) + (f1,)
            tB[i2].after = (tB[i2].after or ()) + (f2,)
        run_tasks(tB)
        ag_m(1)
        run_tasks(dense_tasks(KA, T2tb, QA, T1b, 96, VA, VAb, 0.125, lambda kt: kbias[:, kt % 2:kt % 2 + 1], 0))
        ag_m(0)

        QCD, KCD, VCDT = T1, T2t, T3
        wsrc[0], wsrc[1] = wCD, wCDb
        VC1, VC1b = Vt[0]
        VC4, VC4b = Vt[1]
        VC16, VC16b = Vt[2]
        VD, VDb = Vt[3]
        for c in range(16):
            cs = slice(c * 512, (c + 1) * 512)
            xn, xnb = load_xn(c)
            for gi, (dst, dstb) in enumerate(((QCD, T1b), (KCD, T2tb))):
                p, pb = proj(xn, xnb, 128 * gi, 128)
                rs, rsb = rstd_of(p, pb, 0, 128, BD, 64)
                C.stt("dve", dst[:, cs], p[:, :], gcol[:, 6 + gi:7 + gi], rs[:, :], ALU.mult, ALU.mult,
                      (pb, gcolb, rsb), (dstb,))
            p, pb = proj(xn, xnb, 256, 128)
            C.copy("act", VCDT[:, cs], p[:, :], (pb,), (T3b,))
            vtrans(VCDT, T3b, 0, c * 512, 1, VC1, VC1b, 4 * c, 4)
            vtrans(VCDT, T3b, 64, c * 512, 1, VD, VDb, 4 * c, 4)
            for r in range(4):
                vtrans(VCDT, T3b, 0, c * 512 + r, 4, VC4, VC4b, r * 16 + c, 1)
            if c % 4 == 3:
                s_ = c // 4
                for r in range(16):
                    vtrans(VCDT, T3b, 0, s_ * 2048 + r, 16, VC16, VC16b, r * 4 + s_, 1)

        def local(base, d, r, nb, n0, nblk, V, Vb, vidx, Wc, fin):
            tasks = []
            kts = list(range(max(n0 - 1, 0), n0 + nblk))
            for kt in kts:
                qlo, qhi = max(kt, n0), min(kt + 1, n0 + nblk - 1)
                ncol = (qhi - qlo + 1) * 128
                woff = 0 if qlo == kt else 128
                ka = kt * 128 * d + r
                qa = qlo * 128 * d + r
                t = Task()

                def qk(Sx, Sb, ka=ka, qa=qa, ncol=ncol):
                    C.mm(Sx[:, 0:ncol], KCD[base:base + 64, ka:ka + 127 * d + 1:d],
                         QCD[base:base + 64, qa:qa + (ncol - 1) * d + 1:d], True, True, (T2tb, T1b), (Sb,))

                def ex(Sx, Sb, E, Eb, ncol=ncol, woff=woff):
                    C.act(E[:, 0:ncol], Sx[:, 0:ncol], AF.Exp, (Sb,), (Eb,), scale=0.125)
                    C.tt("dve", E[:, 0:ncol], E[:, 0:ncol], Wt[:, Wc + woff:Wc + woff + ncol], ALU.mult,
                         (Eb, Wtb), (Eb,))

                def pv(E, Eb, O, Ob, kt=kt, qlo=qlo, qhi=qhi):
                    for qb in range(qlo, qhi + 1):
                        first = (kt == kts[0]) and (qb == qlo)
                        last = (kt == kts[-1]) and (qb == qhi)
                        C.mm(O[0:65, (qb - n0) * 128:(qb - n0 + 1) * 128], V[:, vidx(kt), :],
                             E[:, (qb - qlo) * 128:(qb - qlo + 1) * 128], first, last, (Vb, Eb), (Ob,))
                t.qk, t.ex, t.pv = qk, ex, pv
                t.newO = kt == kts[0]
                t.fin = fin if kt == kts[-1] else None
                t.after = None
                tasks.append(t)
            return tasks

        tD = []
        for qt in range(16):
            def finD(O, Ob, qt=qt):
                Osb, Osbb = Osb_r.next()
                C.copy("act", Osb[0:65, :], O[0:65, :], (Ob,), (Osbb,))
                finalize(Osb[0:65, :], Osbb, 3, qt * 512, sink=True)
            tD += local(64, 1, 0, 64, 4 * qt, 4, VD, VDb, lambda kt: kt, 768, finD)
        run_tasks(tD)
        ag_m(3)

        for s_ in range(4):
            tC = []

            def acc(O, Ob, off, d, ncol):
                dst = OCT[0:65, off:off + (ncol - 1) * d + 1:d]
                C.tt("dve", dst, O[0:65, 0:ncol], dst, ALU.add, (Ob, OCTb), (OCTb,))
            for q4 in range(4):
                def fin1(O, Ob, q4=q4):
                    C.copy("act", OCT[0:65, q4 * 512:(q4 + 1) * 512], O[0:65, :], (Ob,),
                           (OCTb,) + tuple(rb for _, rb in raw_items))
                tC += local(0, 1, 0, 64, 16 * s_ + 4 * q4, 4, VC1, VC1b, lambda kt: kt, 0, fin1)
            for r in range(4):
                tC += local(0, 4, r, 16, 4 * s_, 4, VC4, VC4b, lambda kt, r=r: r * 16 + kt, 256,
                            lambda O, Ob, r=r: acc(O, Ob, r, 4, 512))
            for r in range(16):
                tC += local(0, 16, r, 4, s_, 1, VC16, VC16b, lambda kt, r=r: r * 4 + kt, 512,
                            lambda O, Ob, r=r: acc(O, Ob, r, 16, 128))
            run_tasks(tC)
            for q4 in range(4):
                finalize(OCT[0:65, q4 * 512:(q4 + 1) * 512], OCTb, 2, s_ * 2048 + q4 * 512)
        ag_m(2)
        C.P.emit()


def _load(C, shape, dt, src, nm, eng="sp"):
    t, b = C.sb(shape, dt, nm)
    C.dma(eng, t[tuple(slice(None) for _ in shape)], src, (), (b,))
    return t, b


M_LAYER_INPUTS = (("w_o", [D, D], F32), ("gog", [128, 8], F32), ("mlg", [128, 8], F32),
                  ("w_up", [D, 4096], F32), ("w_down", [4096, D], F32))


_NEEDED = set()


def build_fused(nphase=5):
    nc = bass.Bass("TRN2", target_bir_lowering=False)
    with contextlib.ExitStack() as st:
        C = Ctx(nc, st)
        G = {}
        x_d = C.dram("x", [2048, D], F32, "ExternalInput")
        G["ident"] = C.dram("ident", [128, 128], BF16, "ExternalInput")
        for l in range(2):
            if nphase >= 3 + 2 * l:
                for n, shp, dt in M_LAYER_INPUTS:
                    G["%s_%d" % (n, l)] = C.dram("%s_%d" % (n, l), shp, dt, "ExternalInput")
            if nphase >= 2 + 2 * l:
                for n, shp, dt in A_LAYER_INPUTS:
                    G["%s_%d" % (n, l)] = C.dram("%s_%d" % (n, l), shp, dt, "ExternalInput")
        if nphase >= 2:
            for n, shp, dt in A_SHARED_INPUTS:
                G[n] = C.dram(n, shp, dt, "ExternalInput")
        _NEEDED.clear()
        _NEEDED.update(k for k in G)
        _NEEDED.update(("x", "ident"))
        xo_d = C.dram("xo", [2048, D], F32, "ExternalOutput")
        t = nc.dram_tensor("xres", [2048, D], F32)
        G["xres"], G["xres_b"] = t.ap(), Buf("xres", multi=True)
        for n, shp in (("xnT_own", [D, 512]), ("xnT_all", [4 * D, 512]), ("OT_own", [64, S]), ("OT_all", [256, S])):
            ts_ = [nc.dram_tensor("%s%d" % (n, j), list(shp), BF16) for j in range(4)]
            G[n + "_t"] = ts_
            G[n] = [t.ap() for t in ts_]
            G[n + "_b"] = [Buf("%s%d" % (n, j), multi=True) for j in range(4)]
        xin_b, xo_b = Buf("xin"), Buf("xo", multi=True)
        phase_M(C, G, 0, False, True, x_d, xin_b, None, None)
        if nphase >= 2:
            phase_A(C, G, 0)
        if nphase >= 3:
            phase_M(C, G, 0, True, True, x_d, xin_b, G["xres"], G["xres_b"])
        if nphase >= 4:
            phase_A(C, G, 1)
        if nphase >= 5:
            phase_M(C, G, 1, True, False, G["xres"], G["xres_b"], xo_d, xo_b)
        else:
            C.stack = st
            C.dma("sp", xo_d[0:128, :], x_d[0:128, :], (), (xo_b,))
            C.P.emit()
    return nc


_CACHE = {}


def _prog(key, fn):
    if key not in _CACHE:
        _CACHE[key] = fn()
    return _CACHE[key]


def _consts(i):
    f8 = np.float64
    sl_a = 2.0 ** (-8.0 * (3 * i + 1) / 12)
    sl_c = 2.0 ** (-8.0 * (3 * i + 2) / 12)
    sl_d = 2.0 ** (-8.0 * (3 * i + 3) / 12)
    p = np.arange(128, dtype=f8)[:, None]
    j = np.arange(128, dtype=f8)[None, :]
    T2 = (-8.0 * sl_a * (128.0 * (63 - j) + p - 128.0) + BIG).astype(np.float32)
    PM = np.zeros((128, 128), np.float32)
    PM[:, 32:64] = -1e30
    PM[:, 64 + 32] = 1.0
    kbias = np.stack([sl_a * (par * 128 + np.arange(128, dtype=f8) - 128) for par in (0, 1)], 1).astype(np.float32)
    k = np.arange(128)[:, None]
    q = np.arange(128)[None, :]
    cst = np.zeros((128, 512), np.float32)
    cst[:, 0:128] = np.eye(128)
    cst[:, 128:256] = (k <= q)
    cst[:, 256:384] = (k // 64 == q // 64)
    cst[:, 384:512] = 1.0
    Wt = np.zeros((128, 1024), f8)
    for bi, d in enumerate((1, 4, 16)):
        Wt[:, bi * 256:bi * 256 + 128] = (k <= q) * np.exp(-sl_c * d * np.maximum(q - k, 0))
        Wt[:, bi * 256 + 128:bi * 256 + 256] = (k >= q) * np.exp(-sl_c * d * (128 + q - k))
    Wt[:, 768:896] = (k <= q) * np.exp(-sl_d * np.maximum(q - k, 0))
    Wt[:, 896:1024] = (k > q) * np.exp(-sl_d * (128 + q - k))
    SEL = np.zeros((128, 64), np.float32)
    SEL[64, :] = 1.0
    return dict(T2=T2, PM=PM, kbias=kbias, cst=cst.astype(NPBF), Wt=Wt.astype(np.float32).astype(NPBF), SEL=SEL)


def _shared_consts():
    inv = 1.0 / (10000.0 ** (np.arange(0, 32, 2, dtype=np.float32) / 32))
    ang = np.arange(S, dtype=np.float32)[:, None] * inv[None, :]
    cos, sin = np.cos(ang).T, np.sin(ang).T
    rope = np.concatenate([cos, cos, -sin, sin], 0).astype(np.float32)
    OH = (np.arange(S)[None, :] // 256 == np.arange(32)[:, None]).astype(np.float32).astype(NPBF)
    return np.ascontiguousarray(rope), np.ascontiguousarray(OH)


def _col(v, n=128):
    o = np.zeros((128,), np.float32)
    o[:len(v)] = v
    return o


def _attn_inputs(P, l, i, xnT_b, rope, OH):
    w_in = P["w_in"][l]
    kv = i // 2
    sel = lambda a, n=64: list(range(a, a + n))
    krp = list(range(1152 + 16, 1184)) + list(range(1152, 1168))
    colsAB = (sel(64 * i) + krp + sel(256 + 64 * i) + sel(512 + 64 * i) + sel(1152, 32)
              + sel(768, 256) + sel(1024, 128))
    colsCD = (sel(1184 + 64 * i) + sel(1952 + 64 * i) + sel(1440 + 64 * i) + sel(2208 + 64 * kv)
              + sel(1696 + 64 * i) + sel(2336 + 64 * kv))
    uq = P["mla_w_uq"][l][:, 96 * i:96 * i + 96]
    perm = list(range(80, 96)) + list(range(64, 80))
    wuq = np.concatenate([uq, uq[:, 0:64], uq[:, perm]], 1)
    gq, gk = P["mla_q_g"][l], P["mla_k_g"][l]
    gqp, gkp = np.zeros(96, np.float32), np.zeros(96, np.float32)
    gqp[64:96], gkp[64:96] = gq[perm], gk[perm]
    gcol = np.stack([_col(P["moba_q_g"][l]), _col(P["moba_k_g"][l]), _col(gq), _col(gqp), _col(gk), _col(gkp),
                     np.concatenate([P["dil_q_g"][l], P["swa_q_g"][l]]),
                     np.concatenate([P["dil_k_g"][l], P["swa_k_g"][l]])], 1).astype(np.float32)
    d = dict(ang=np.ascontiguousarray(P["attn_norm_g"][l].reshape(8, 128).T),
             wAB=np.ascontiguousarray(w_in[:, colsAB]), wCD=np.ascontiguousarray(w_in[:, colsCD]),
             wuq=np.ascontiguousarray(wuq), gql=np.ascontiguousarray(P["mla_qlat_g"][l].reshape(2, 128).T),
             wukv=np.ascontiguousarray(P["mla_w_ukv"][l][:, 128 * i:128 * i + 128]),
             gkvl=np.ascontiguousarray(P["mla_kvlat_g"][l].reshape(128, 1)), gcol=np.ascontiguousarray(gcol),
             rope=rope, OH=OH, sink=np.full((128, 1), P["swa_sinks"][l][i], np.float32))
    d.update(_consts(i))
    return d


def kernel(**inputs):
    P = {k: np.asarray(v) for k, v in inputs.items()}
    x = np.ascontiguousarray(P["x"], dtype=np.float32)
    cores = list(range(NCORES))
    ident = np.eye(128, dtype=np.float32).astype(NPBF)
    rope, OH = _shared_consts()
    nc = _prog("fused", build_fused)
    ims = []
    for c in cores:
        b, i = c // 4, c % 4
        d = {"x": np.ascontiguousarray(x[b, 2048 * i:2048 * (i + 1)]), "ident": ident}
        for l in range(2):
            a = _attn_inputs(P, l, i, None, rope, OH)
            for n, _, _ in A_LAYER_INPUTS:
                d["%s_%d" % (n, l)] = a[n]
            for n, _, _ in A_SHARED_INPUTS:
                d[n] = a[n]
            d["w_o_%d" % l] = P["w_o"][l]
            d["gog_%d" % l] = np.ascontiguousarray(P["group_out_g"][l].reshape(8, 128).T)
            d["mlg_%d" % l] = np.ascontiguousarray(P["mlp_norm_g"][l].reshape(8, 128).T)
            d["w_up_%d" % l] = P["w_up"][l]
            d["w_down_%d" % l] = P["w_down"][l]
        ims.append({k: v for k, v in d.items() if k in _NEEDED})
    res = run_bass_kernel_spmd(nc, ims, core_ids=cores)
    out = np.zeros((2, S, D), np.float32)
    for c in cores:
        out[c // 4, 2048 * (c % 4):2048 * (c % 4 + 1)] = np.asarray(res.results[c]["xo"])
    return out
```
